# Optimizing a Trainium2 kernel written in Bass

```python
import jax, jax.numpy as jnp
from jax import lax
import numpy as np

D_MODEL = 2048
BATCH = 4
SEQ = 2048
DEPTH = 4

D_MIX = D_MODEL
N_MIXERS = 4
W_BR = D_MIX // N_MIXERS
HEAD_DIM = 128
N_HEADS_BR = W_BR // HEAD_DIM
GLA_DK = W_BR // 2
GLA_HEAD_K = GLA_DK // N_HEADS_BR
GLA_LOWRANK = 16
GLA_TAU = 16.0
GLA_CHUNK = 64
LRU_C = 8.0
LRU_CONV = 4
NSA_DH = HEAD_DIM
CMP_LEN = 32
CMP_STRIDE = 16
CMP_HIDDEN = 128
SLC_LEN = 64
SLC_TOPN = 16
SLC_Q_BLOCK = 64
WIN = 512
Q_BLOCK = 128
WIN_BLOCKS = WIN // Q_BLOCK
FORCED_SCORE = 1e3
CONF_KERNEL = 31
EPS = 1e-6

PROJ_SIZES = (
    GLA_DK, GLA_DK, W_BR, GLA_LOWRANK, W_BR,
    W_BR, W_BR,
    W_BR, NSA_DH, NSA_DH, NSA_DH, NSA_DH, NSA_DH, NSA_DH,
    3 * N_HEADS_BR, W_BR,
    W_BR, W_BR, W_BR,
)
D_PROJ = sum(PROJ_SIZES)

kernel_name = 'hybrid_gla_rglru_nsa_conformer_trunk'

F32 = jnp.float32


def rmsnorm(x, g):
    x32 = x.astype(F32)
    y = x32 * lax.rsqrt(jnp.mean(x32 * x32, axis=-1, keepdims=True) + EPS)
    return (y * g.astype(F32)).astype(x.dtype)


def group_rmsnorm(y, g):
    shp = y.shape
    y32 = y.astype(F32).reshape(shp[:-1] + (shp[-1] // HEAD_DIM, HEAD_DIM))
    y32 = y32 * lax.rsqrt(jnp.mean(y32 * y32, axis=-1, keepdims=True) + EPS)
    return (y32.reshape(shp) * g.astype(F32)).astype(y.dtype)


def layernorm(x, g, b):
    x32 = x.astype(F32)
    mu = jnp.mean(x32, axis=-1, keepdims=True)
    xc = x32 - mu
    var = jnp.mean(xc * xc, axis=-1, keepdims=True)
    return (xc * lax.rsqrt(var + EPS) * g.astype(F32) + b.astype(F32)).astype(x.dtype)


def causal_dwconv(x, w, b):
    width, ch = w.shape
    y = lax.conv_general_dilated(x, w[:, None, :].astype(x.dtype), window_strides=(1,),
                                 padding=((width - 1, 0),),
                                 dimension_numbers=('NWC', 'WIO', 'NWC'),
                                 feature_group_count=ch)
    return y + b.astype(x.dtype)


def masked_softmax(s, mask):
    s = jnp.where(mask, s.astype(F32), -jnp.inf)
    m = jnp.max(s, axis=-1, keepdims=True)
    m = jnp.where(jnp.isfinite(m), m, 0.0)
    p = jnp.exp(s - m)
    return p / jnp.maximum(jnp.sum(p, axis=-1, keepdims=True), 1e-30)


def gla_mixer(q, k, v, fg, w_fg2, b_fg2):
    B, S, _ = q.shape
    H, C = N_HEADS_BR, GLA_CHUNK
    N = S // C

    def chunks(t, d):
        return t.reshape(B, N, C, H, d).transpose(0, 3, 1, 2, 4)

    log_f = jax.nn.log_sigmoid((fg @ w_fg2 + b_fg2).astype(F32)) / GLA_TAU
    qc = chunks(q.astype(F32) * GLA_HEAD_K ** -0.5, GLA_HEAD_K)
    kc = chunks(k.astype(F32), GLA_HEAD_K)
    vc = chunks(v.astype(F32), HEAD_DIM)
    bcum = jnp.cumsum(chunks(log_f, GLA_HEAD_K), axis=3)
    b_last = bcum[:, :, :, -1:, :]
    q_dec = qc * jnp.exp(bcum)
    causal = jnp.tril(jnp.ones((C, C), dtype=bool))
    attn = jnp.where(causal, jnp.einsum('bhncd,bhnjd->bhncj', q_dec, kc * jnp.exp(-bcum)), 0.0)
    o_intra = jnp.einsum('bhncj,bhnje->bhnce', attn, vc)
    dstate = jnp.einsum('bhncd,bhnce->bhnde', kc * jnp.exp(b_last - bcum), vc)
    decay = jnp.exp(b_last[:, :, :, 0, :])

    def step(state, inp):
        dec, ds = inp
        return dec[..., None] * state + ds, state

    _, s_prev = lax.scan(step, jnp.zeros((B, H, GLA_HEAD_K, HEAD_DIM), F32),
                         (jnp.moveaxis(decay, 2, 0), jnp.moveaxis(dstate, 2, 0)))
    s_prev = jnp.moveaxis(s_prev, 0, 2)
    o = o_intra + jnp.einsum('bhncd,bhnde->bhnce', q_dec, s_prev)
    return o.transpose(0, 2, 3, 1, 4).reshape(B, S, H * HEAD_DIM)


def rglru_mixer(xb, conv_w, conv_b, w_a, b_a, w_x, b_x, lam):
    B, S, W = xb.shape
    xc = causal_dwconv(xb, conv_w, conv_b)
    xh = xc.reshape(B, S, N_HEADS_BR, HEAD_DIM)
    r = jax.nn.sigmoid((jnp.einsum('bshi,hij->bshj', xh, w_a).reshape(B, S, W) + b_a).astype(F32))
    ig = jax.nn.sigmoid((jnp.einsum('bshi,hij->bshj', xh, w_x).reshape(B, S, W) + b_x).astype(F32))
    log_a = -LRU_C * r * jax.nn.softplus(-lam.astype(F32))
    a = jnp.exp(log_a)
    u = jnp.sqrt(-jnp.expm1(2.0 * log_a)) * (ig * xc.astype(F32))

    def combine(left, right):
        a1, b1 = left
        a2, b2 = right
        return a1 * a2, a2 * b1 + b2

    _, h = lax.associative_scan(combine, (a, u), axis=1)
    return h


def nsa_mixer(q, kc, vc, ks, vs, kw, vw, gates, pos_k, w1_k, w2_k, pos_v, w1_v, w2_v):
    B, S, _ = q.shape
    H, Dh = N_HEADS_BR, NSA_DH
    scale = Dh ** -0.5
    qh = q.reshape(B, S, H, Dh)
    t = jnp.arange(S)

    n_cmp = (S - CMP_LEN) // CMP_STRIDE + 1
    cstart = jnp.arange(n_cmp) * CMP_STRIDE
    blk = cstart[:, None] + jnp.arange(CMP_LEN)[None]

    def compress(z, pos, w1, w2):
        zb = (z[:, blk] + pos).reshape(B, n_cmp, CMP_LEN * Dh)
        return jax.nn.silu(zb @ w1) @ w2

    k_cmp = compress(kc, pos_k, w1_k, w2_k)
    v_cmp = compress(vc, pos_v, w1_v, w2_v)
    cmp_mask = (t[:, None] >= (cstart + CMP_LEN - 1)[None])[None, None]
    p_cmp = masked_softmax(jnp.einsum('bshd,bnd->bhsn', qh, k_cmp) * scale, cmp_mask)
    o_cmp = jnp.einsum('bhsn,bnd->bshd', p_cmp, v_cmp)

    n_sel = S // SLC_LEN
    j = jnp.arange(n_sel)
    overlap = ((cstart[:, None] < (j[None] + 1) * SLC_LEN) &
               (cstart[:, None] + CMP_LEN > j[None] * SLC_LEN)).astype(F32)
    imp = jnp.einsum('bhsn,nj->bsj', p_cmp, overlap)
    cur = (t // SLC_LEN)[:, None]
    forced = (j[None] == 0) | (j[None] == cur) | (j[None] == cur - 1)
    imp = jnp.where(j[None] > cur, -jnp.inf, jnp.where(forced, FORCED_SCORE, imp))
    n_top = min(SLC_TOPN, n_sel)
    _, sel = lax.top_k(imp, n_top)

    ks_blk = ks.reshape(B, n_sel, SLC_LEN, Dh)
    vs_blk = vs.reshape(B, n_sel, SLC_LEN, Dh)
    nq = S // SLC_Q_BLOCK

    def sel_block(args):
        q_b, sel_b, t_b = args
        kg = jax.vmap(lambda kb, ib: kb[ib])(ks_blk, sel_b)
        vg = jax.vmap(lambda vb, ib: vb[ib])(vs_blk, sel_b)
        kpos = sel_b[..., None] * SLC_LEN + jnp.arange(SLC_LEN)
        mask = (kpos <= t_b[None, :, None, None]).reshape(B, SLC_Q_BLOCK, 1, n_top * SLC_LEN)
        s = jnp.einsum('bqhd,bqkld->bqhkl', q_b, kg).reshape(B, SLC_Q_BLOCK, H, n_top * SLC_LEN) * scale
        p = masked_softmax(s, mask).reshape(B, SLC_Q_BLOCK, H, n_top, SLC_LEN)
        return jnp.einsum('bqhkl,bqkld->bqhd', p, vg)

    o_slc = lax.map(sel_block, (qh.reshape(B, nq, SLC_Q_BLOCK, H, Dh).swapaxes(0, 1),
                                sel.reshape(B, nq, SLC_Q_BLOCK, n_top).swapaxes(0, 1),
                                t.reshape(nq, SLC_Q_BLOCK)))
    o_slc = o_slc.swapaxes(0, 1).reshape(B, S, H, Dh)

    nb = S // Q_BLOCK
    pad = WIN_BLOCKS * Q_BLOCK

    def band(z):
        zb = jnp.pad(z, ((0, 0), (pad, 0), (0, 0))).reshape(B, nb + WIN_BLOCKS, Q_BLOCK, Dh)
        return jnp.concatenate([zb[:, i:i + nb] for i in range(WIN_BLOCKS + 1)], axis=2)

    kwin, vwin = band(kw), band(vw)
    qpos = t.reshape(nb, Q_BLOCK)
    kpos = (jnp.arange(nb)[:, None] - WIN_BLOCKS) * Q_BLOCK + jnp.arange((WIN_BLOCKS + 1) * Q_BLOCK)[None]
    dist = qpos[:, :, None] - kpos[:, None, :]
    wmask = (dist >= 0) & (dist < WIN) & (kpos[:, None, :] >= 0)
    s = jnp.einsum('bnqhd,bnkd->bnhqk', qh.reshape(B, nb, Q_BLOCK, H, Dh), kwin) * scale
    p = masked_softmax(s, wmask[None, :, None])
    o_win = jnp.einsum('bnhqk,bnkd->bnqhd', p, vwin).reshape(B, S, H, Dh)

    g = jax.nn.sigmoid(gates.astype(F32)).reshape(B, S, H, 3)
    o = g[..., 0:1] * o_cmp + g[..., 1:2] * o_slc + g[..., 2:3] * o_win
    return o.reshape(B, S, H * Dh)


def conformer_mixer(val, glu, dw_w, dw_b, ln_g, ln_b, pw_w, pw_b):
    y = val * jax.nn.sigmoid(glu)
    y = causal_dwconv(y, dw_w, dw_b)
    y = layernorm(y, ln_g, ln_b)
    return jax.nn.silu(y) @ pw_w + pw_b


def hybrid_layer(x, pre_g, w_in, gla_w_fg2, gla_b_fg2, lru_conv_w, lru_conv_b, lru_w_a, lru_b_a,
                 lru_w_x, lru_b_x, lru_lambda, cmp_pos_k, cmp_w1_k, cmp_w2_k, cmp_pos_v, cmp_w1_v,
                 cmp_w2_v, conf_dw_w, conf_dw_b, conf_ln_g, conf_ln_b, conf_pw_w, conf_pw_b,
                 branch_g, w_out, post_g):
    dt = x.dtype
    h = rmsnorm(x, pre_g)
    proj = h @ w_in
    (gla_q, gla_k, gla_v, gla_fg, gla_z, lru_x, lru_z, nsa_q, nsa_kc, nsa_vc, nsa_ks, nsa_vs,
     nsa_kw, nsa_vw, nsa_g, nsa_z, conv_v, conv_glu, conv_z) = jnp.split(
        proj, np.cumsum(PROJ_SIZES)[:-1].tolist(), axis=-1)
    o_a = gla_mixer(gla_q, gla_k, gla_v, gla_fg, gla_w_fg2, gla_b_fg2)
    o_b = rglru_mixer(lru_x, lru_conv_w, lru_conv_b, lru_w_a, lru_b_a, lru_w_x, lru_b_x, lru_lambda)
    o_c = nsa_mixer(nsa_q, nsa_kc, nsa_vc, nsa_ks, nsa_vs, nsa_kw, nsa_vw, nsa_g,
                    cmp_pos_k, cmp_w1_k, cmp_w2_k, cmp_pos_v, cmp_w1_v, cmp_w2_v)
    o_d = conformer_mixer(conv_v, conv_glu, conf_dw_w, conf_dw_b, conf_ln_g, conf_ln_b, conf_pw_w, conf_pw_b)
    mixed = jnp.concatenate([o_a.astype(dt), o_b.astype(dt), o_c.astype(dt), o_d.astype(dt)], axis=-1)
    gate = jax.nn.silu(jnp.concatenate([gla_z, lru_z, nsa_z, conv_z], axis=-1))
    mixed = (group_rmsnorm(mixed, branch_g) * gate).astype(dt)
    y = mixed @ w_out
    return x + rmsnorm(y, post_g)


def setup_inputs(seed: int = 0) -> dict:
    key = jax.random.key(seed)
    keys = iter(jax.random.split(key, 32))
    L = DEPTH

    def nrm(shape, s):
        return jax.random.normal(next(keys), shape, F32) * s

    def gain(shape):
        return 1.0 + nrm(shape, 0.02)

    x = nrm((BATCH, SEQ, D_MODEL), 1.0)
    pre_norm_g = gain((L, D_MODEL))
    w_in = nrm((L, D_MODEL, D_PROJ), D_MODEL ** -0.5)
    gla_w_fg2 = nrm((L, GLA_LOWRANK, GLA_DK), GLA_LOWRANK ** -0.5)
    gla_b_fg2 = nrm((L, GLA_DK), 0.01)
    lru_conv_w = nrm((L, LRU_CONV, W_BR), LRU_CONV ** -0.5)
    lru_conv_b = nrm((L, W_BR), 0.01)
    lru_w_a = nrm((L, N_HEADS_BR, HEAD_DIM, HEAD_DIM), HEAD_DIM ** -0.5)
    lru_b_a = nrm((L, W_BR), 0.01)
    lru_w_x = nrm((L, N_HEADS_BR, HEAD_DIM, HEAD_DIM), HEAD_DIM ** -0.5)
    lru_b_x = nrm((L, W_BR), 0.01)
    u = jax.random.uniform(next(keys), (L, W_BR), F32, 0.9, 0.999)
    a0 = u ** (1.0 / LRU_C)
    lru_lambda = jnp.log(a0) - jnp.log1p(-a0)
    nsa_cmp_pos_k = nrm((L, CMP_LEN, NSA_DH), 0.02)
    nsa_cmp_w1_k = nrm((L, CMP_LEN * NSA_DH, CMP_HIDDEN), (CMP_LEN * NSA_DH) ** -0.5)
    nsa_cmp_w2_k = nrm((L, CMP_HIDDEN, NSA_DH), CMP_HIDDEN ** -0.5)
    nsa_cmp_pos_v = nrm((L, CMP_LEN, NSA_DH), 0.02)
    nsa_cmp_w1_v = nrm((L, CMP_LEN * NSA_DH, CMP_HIDDEN), (CMP_LEN * NSA_DH) ** -0.5)
    nsa_cmp_w2_v = nrm((L, CMP_HIDDEN, NSA_DH), CMP_HIDDEN ** -0.5)
    conf_dw_w = nrm((L, CONF_KERNEL, W_BR), CONF_KERNEL ** -0.5)
    conf_dw_b = nrm((L, W_BR), 0.01)
    conf_ln_g = gain((L, W_BR))
    conf_ln_b = nrm((L, W_BR), 0.01)
    conf_pw_w = nrm((L, W_BR, W_BR), W_BR ** -0.5)
    conf_pw_b = nrm((L, W_BR), 0.01)
    branch_norm_g = gain((L, D_MIX))
    w_out = nrm((L, D_MIX, D_MODEL), D_MIX ** -0.5)
    post_norm_g = gain((L, D_MODEL))
    return {'x': x, 'pre_norm_g': pre_norm_g, 'w_in': w_in, 'gla_w_fg2': gla_w_fg2,
            'gla_b_fg2': gla_b_fg2, 'lru_conv_w': lru_conv_w, 'lru_conv_b': lru_conv_b,
            'lru_w_a': lru_w_a, 'lru_b_a': lru_b_a, 'lru_w_x': lru_w_x, 'lru_b_x': lru_b_x,
            'lru_lambda': lru_lambda, 'nsa_cmp_pos_k': nsa_cmp_pos_k, 'nsa_cmp_w1_k': nsa_cmp_w1_k,
            'nsa_cmp_w2_k': nsa_cmp_w2_k, 'nsa_cmp_pos_v': nsa_cmp_pos_v, 'nsa_cmp_w1_v': nsa_cmp_w1_v,
            'nsa_cmp_w2_v': nsa_cmp_w2_v, 'conf_dw_w': conf_dw_w, 'conf_dw_b': conf_dw_b,
            'conf_ln_g': conf_ln_g, 'conf_ln_b': conf_ln_b, 'conf_pw_w': conf_pw_w,
            'conf_pw_b': conf_pw_b, 'branch_norm_g': branch_norm_g, 'w_out': w_out,
            'post_norm_g': post_norm_g}


def reference(x, pre_norm_g, w_in, gla_w_fg2, gla_b_fg2, lru_conv_w, lru_conv_b, lru_w_a, lru_b_a,
              lru_w_x, lru_b_x, lru_lambda, nsa_cmp_pos_k, nsa_cmp_w1_k, nsa_cmp_w2_k, nsa_cmp_pos_v,
              nsa_cmp_w1_v, nsa_cmp_w2_v, conf_dw_w, conf_dw_b, conf_ln_g, conf_ln_b, conf_pw_w,
              conf_pw_b, branch_norm_g, w_out, post_norm_g):
    for l in range(DEPTH):
        x = hybrid_layer(x, pre_norm_g[l], w_in[l], gla_w_fg2[l], gla_b_fg2[l], lru_conv_w[l],
                         lru_conv_b[l], lru_w_a[l], lru_b_a[l], lru_w_x[l], lru_b_x[l], lru_lambda[l],
                         nsa_cmp_pos_k[l], nsa_cmp_w1_k[l], nsa_cmp_w2_k[l], nsa_cmp_pos_v[l],
                         nsa_cmp_w1_v[l], nsa_cmp_w2_v[l], conf_dw_w[l], conf_dw_b[l], conf_ln_g[l],
                         conf_ln_b[l], conf_pw_w[l], conf_pw_b[l], branch_norm_g[l], w_out[l],
                         post_norm_g[l])
    return x
```

```python
from contextlib import ExitStack
import numpy as np
import concourse.bass as bass
import concourse.mybir as mybir
from concourse.bass_utils import run_bass_kernel_spmd

F32 = mybir.dt.float32
BF16 = mybir.dt.bfloat16
ALU = mybir.AluOpType
AF = mybir.ActivationFunctionType
AX = mybir.AxisListType

S = 2048
D = 2048
NT = 16
DPROJ = 5916
BIG = 30000.0
NEG = -1.0e30

WNAMES = [
    ("pre_norm_g", [D]), ("w_in", [D, DPROJ]), ("gla_w_fg2", [16, 256]), ("gla_b_fg2", [256]),
    ("lru_conv_w", [4, 512]), ("lru_conv_b", [512]), ("lru_w_a", [4, 128, 128]), ("lru_b_a", [512]),
    ("lru_w_x", [4, 128, 128]), ("lru_b_x", [512]), ("lru_lambda", [512]),
    ("nsa_cmp_pos_k", [32, 128]), ("nsa_cmp_w1_k", [4096, 128]), ("nsa_cmp_w2_k", [128, 128]),
    ("nsa_cmp_pos_v", [32, 128]), ("nsa_cmp_w1_v", [4096, 128]), ("nsa_cmp_w2_v", [128, 128]),
    ("conf_dw_w", [31, 512]), ("conf_dw_b", [512]), ("conf_ln_g", [512]), ("conf_ln_b", [512]),
    ("conf_pw_w", [512, 512]), ("conf_pw_b", [512]), ("branch_norm_g", [D]), ("w_out", [D, D]),
    ("post_norm_g", [D]),
]


class Buf:
    __slots__ = ("name", "w", "r")

    def __init__(self, name=""):
        self.name = name
        self.w = None
        self.r = []


class Prog:
    EPOCH = 2000
    NDS = 10

    def __init__(self, nc):
        self.nc = nc
        self.engs = ["pe", "act", "dve", "pool", "sp"]
        self.ops = {e: [] for e in self.engs}
        self.cnt = {e: 0 for e in self.engs}
        self.dma_n = {"sp": 0, "pool": 0, "act": 0}
        self.waited = {e: {} for e in self.engs}
        self.last = {}

    def _deps(self, eng, reads, writes):
        deps = []
        for b in reads:
            if b.w is not None:
                deps.append(b.w)
        for b in writes:
            if b.w is not None:
                deps.append(b.w)
            for ev in b.r:
                if ev[0][0] != eng:
                    deps.append(ev)
        return deps

    def _update(self, reads, writes, ev):
        for b in reads:
            b.r.append(ev)
        for b in writes:
            b.w = ev
            b.r = []

    def _record(self, eng, deps, fn, ev, inc):
        w = self.waited[eng]
        need = {}
        for (k, v) in deps:
            if eng == "pe" and k[0] == "pe":
                continue
            if w.get(k, 0) >= v:
                continue
            if need.get(k, 0) < v:
                need[k] = v
        for k, v in need.items():
            w[k] = v
        self.ops[eng].append((list(need.items()), fn, ev[0] if ev else None, inc))
        if ev:
            self.last[ev[0]] = ev[1]

    def op(self, eng, fn, reads=(), writes=()):
        deps = self._deps(eng, reads, writes)
        c = self.cnt[eng]
        self.cnt[eng] = c + 1
        ev = ((eng, c // self.EPOCH), c % self.EPOCH + 1)
        self._record(eng, deps, fn, ev, 1)
        self._update(reads, writes, ev)

    def dma(self, q, fn, reads=(), writes=()):
        n = self.dma_n[q]
        self.dma_n[q] = n + 1
        slot = n % self.NDS
        val = 16 * (n // self.NDS + 1)
        key = ("d" + q, slot)
        deps = self._deps(q, reads, writes)
        if val > 16:
            deps.append((key, val - 16))
        ev = (key, val)
        self._record(q, deps, fn, ev, 16)
        self._update(reads, writes, ev)

    def barrier(self):
        evs = list(self.last.items())
        for e in self.engs:
            self._record(e, evs, None, None, 0)

    def emit(self):
        nc = self.nc
        keys = set()
        for e in self.engs:
            for waits, fn, key, inc in self.ops[e]:
                if key is not None:
                    keys.add(key)
                for k, v in waits:
                    keys.add(k)
        sems = {}
        with ExitStack() as st:
            for k in sorted(keys, key=str):
                sems[k] = st.enter_context(nc.semaphore("s_" + str(k[0]) + "_" + str(k[1])))
            blk = st.enter_context(nc.Block())

            def mk(en):
                def body(e):
                    for waits, fn, key, inc in self.ops[en]:
                        for (k, v) in waits:
                            e.wait_ge(sems[k], v)
                        if fn is not None:
                            fn(e).then_inc(sems[key], inc)
                return body

            blk.tensor(mk("pe"))
            blk.scalar(mk("act"))
            blk.vector(mk("dve"))
            blk.gpsimd(mk("pool"))
            blk.sync(mk("sp"))


class Builder:
    def __init__(self, nc, L, upto=99, dbg=False):
        self.nc = nc
        self.L = L
        self.upto = upto
        self.dbg = dbg
        self.P = Prog(nc)
        self.rr = 0

    def alloc(self, shape, dt=BF16):
        n = int(np.prod(shape))
        nb = n * (2 if dt == F32 else 1)
        nb = (nb + 1) // 2 * 2
        assert self.off + nb <= self.ACOLS, ("arena overflow", self.off, nb)
        a = self.arena[:, self.off:self.off + nb]
        self.off += nb
        if dt == F32:
            a = a.bitcast(F32)
        if len(shape) == 2:
            a = a.rearrange("p (a b) -> p a b", a=shape[0])
        elif len(shape) == 3:
            a = a.rearrange("p (a b c) -> p a b c", a=shape[0], b=shape[1])
        return a

    def mm(self, out, lhsT, rhs, start, stop, r, w, **kw):
        self.P.op("pe", lambda e: e.matmul(out, lhsT=lhsT, rhs=rhs, start=start, stop=stop, **kw), reads=r, writes=w)

    def tr(self, out, in_, ident, r, w):
        self.P.op("pe", lambda e: e.transpose(out=out, in_=in_, identity=ident), reads=r, writes=w)

    def act(self, out, in_, func, r, w, **kw):
        self.P.op("act", lambda e: e.activation(out=out, in_=in_, func=func, **kw), reads=r, writes=w)

    def cp(self, eng, out, in_, r, w):
        if eng == "act":
            self.act(out, in_, AF.Copy, r, w)
        else:
            self.P.op(eng, lambda e: e.tensor_copy(out=out, in_=in_), reads=r, writes=w)

    def evac(self, out, in_, r, w):
        self.rr ^= 1
        self.cp("act" if self.rr else "dve", out, in_, r, w)

    def tt(self, eng, out, in0, in1, op, r, w):
        self.P.op(eng, lambda e: e.tensor_tensor(out=out, in0=in0, in1=in1, op=op), reads=r, writes=w)

    def ts(self, eng, out, in0, s1, s2, op0, op1, r, w):
        if op1 is None:
            self.P.op(eng, lambda e: e.tensor_scalar(out=out, in0=in0, scalar1=s1, scalar2=None, op0=op0), reads=r, writes=w)
        else:
            self.P.op(eng, lambda e: e.tensor_scalar(out=out, in0=in0, scalar1=s1, scalar2=s2, op0=op0, op1=op1), reads=r, writes=w)

    def stt(self, eng, out, in0, scalar, in1, op0, op1, r, w):
        self.P.op(eng, lambda e: e.scalar_tensor_tensor(out=out, in0=in0, scalar=scalar, in1=in1, op0=op0, op1=op1), reads=r, writes=w)

    def memset(self, eng, ap, v, w):
        self.P.op(eng, lambda e: e.memset(ap, v), writes=w)

    def asel(self, out, in_, pattern, cmp, fill, base, cm, r, w):
        self.P.op("pool", lambda e: e.affine_select(out=out, in_=in_, pattern=pattern, compare_op=cmp, fill=fill, base=base, channel_multiplier=cm), reads=r, writes=w)

    def dma(self, q, out, in_, r, w):
        self.P.dma(q, lambda e: e.dma_start(out=out, in_=in_), reads=r, writes=w)

    def rsqrt(self, t, r_w):
        self.act(t, t, AF.Ln, [r_w], [r_w])
        self.act(t, t, AF.Exp, [r_w], [r_w], scale=-0.5)

    def build(self):
        nc = self.nc
        L = self.L
        self.x_d = nc.dram_tensor("x", [S, D], F32, kind="ExternalInput").ap()
        self.w = {}
        for n, shp in WNAMES:
            self.w[n] = nc.dram_tensor(n, [L] + shp, F32, kind="ExternalInput").ap()
        self.out_d = nc.dram_tensor("out", [S, D], F32, kind="ExternalOutput").ap()
        self.mixT_d = nc.dram_tensor("mixT", [D, S], BF16, kind=("ExternalOutput" if self.dbg else "Internal")).ap()
        self.B_out = [Buf("out%d" % t) for t in range(NT)]
        self.B_mixd = [Buf("mixd%d" % i) for i in range(4)]
        with ExitStack() as st:
            self.ACOLS = 106200
            self.arena = st.enter_context(nc.sbuf_tensor("arena", [128, self.ACOLS], BF16))
            self.ps = [st.enter_context(nc.psum_tensor("ps%d" % i, [128, 512], F32))[:] for i in range(8)]
            self.off = 0
            self.consts()
            self.base_off = self.off
            for l in range(L):
                self.layer(l)
            self.P.barrier()
            self.P.emit()
        return nc

    def consts(self):
        A = self.alloc
        self.identf = A([128], F32)
        self.ident = A([128], BF16)
        self.ones = A([128], BF16)
        self.triI = A([128], F32)
        self.triS = A([128], F32)
        self.mask4 = A([4, 128], F32)
        self.hT = A([16, S], BF16)
        self.wbuf = [A([16, 512], BF16), A([16, 512], BF16)]
        self.gvec = A([D], F32)
        self.bgbc = A([1024], F32)
        self.rows = A([512], F32)
        self.colv = A([4, 48], F32)
        self.B_c = Buf("consts")
        self.B_hT = [Buf("hT%d" % t) for t in range(NT)]
        self.B_w = [Buf("w0"), Buf("w1")]
        self.B_g = Buf("gvec")
        self.B_bg = Buf("bgbc")
        self.B_rows = Buf("rows")
        self.B_colv = Buf("colv")
        self.B_ps = [Buf("ps%d" % i) for i in range(8)]
        self.wslot = 0
        c = [self.B_c]
        self.memset("pool", self.identf, 0.0, c)
        self.asel(self.identf, self.identf, [[-1, 128]], ALU.not_equal, 1.0, 0, 1, c, c)
        self.cp("pool", self.ident, self.identf, c, c)
        self.memset("pool", self.ones, 1.0, c)
        self.memset("pool", self.triI, 1.0, c)
        self.asel(self.triI, self.triI, [[1, 128]], ALU.is_ge, 0.0, 0, -1, c, c)
        self.memset("pool", self.triS, 1.0, c)
        self.asel(self.triS, self.triS, [[-1, 128]], ALU.is_ge, 0.0, -1, 1, c, c)
        for h in range(4):
            self.cp("pool", self.mask4[:, h, :], self.triI, c, c)
        self.P.barrier()

    def prefetch(self, src, ncols, key):
        self.pref = (key, self.load_w(src, ncols))

    def load_w(self, src, ncols, key=None):
        if key is not None and getattr(self, "pref", None) is not None and self.pref[0] == key:
            r = self.pref[1]
            self.pref = None
            return r
        s = self.wslot
        self.wslot ^= 1
        dst = self.wbuf[s][:, :, 0:ncols]
        self.dma("pool", dst, src.rearrange("(k p) c -> p k c", p=128), [], [self.B_w[s]])
        return self.wbuf[s], self.B_w[s]

    def proj_fm(self, wap, wb, c0, m, dst_fn, pbase, func=AF.Copy, scale=1.0):
        banks = [pbase + i for i in range(4)]
        for kc in range(16):
            for tb in range(4):
                self.mm(self.ps[banks[tb]][0:m, :], wap[:, kc, c0:c0 + m], self.hT[:, kc, tb * 512:(tb + 1) * 512],
                        kc == 0, kc == 15, [wb] + self.B_hT[tb * 4:tb * 4 + 4], [self.B_ps[banks[tb]]])
        for tb in range(4):
            out, ob = dst_fn(tb)
            if func == AF.Copy and scale == 1.0:
                self.evac(out, self.ps[banks[tb]][0:m, :], [self.B_ps[banks[tb]]], [ob])
            else:
                self.act(out, self.ps[banks[tb]][0:m, :], func, [self.B_ps[banks[tb]]], [ob], scale=scale)

    def proj_tm(self, wap, wb, c0, n, dst_fn, func=AF.Copy):
        for tt in range(NT):
            b = tt % 4
            for kc in range(16):
                self.mm(self.ps[b][:, 0:n], self.hT[:, kc, tt * 128:(tt + 1) * 128], wap[:, kc, c0:c0 + n],
                        kc == 0, kc == 15, [wb, self.B_hT[tt]], [self.B_ps[b]])
            out, ob = dst_fn(tt)
            if func == AF.Copy:
                self.evac(out, self.ps[b][:, 0:n], [self.B_ps[b]], [ob])
            else:
                self.act(out, self.ps[b][:, 0:n], func, [self.B_ps[b]], [ob])

    def layer(self, l):
        P = self.P
        W = {n: self.w[n][l] for n, _ in WNAMES}
        self.W = W
        src_d = self.x_d if l == 0 else self.out_d
        self.off = self.base_off
        A = self.alloc
        self.dma("sp", self.gvec, W["pre_norm_g"].partition_broadcast(128), [], [self.B_g])
        self.dma("sp", self.bgbc[:, 0:512], W["branch_norm_g"][0:512].partition_broadcast(128), [], [self.B_bg])
        self.dma("sp", self.bgbc[:, 512:1024], W["branch_norm_g"][1024:1536].partition_broadcast(128), [], [self.B_bg])
        rowsrc = [("lru_conv_w", 4), ("lru_conv_b", 1), ("lru_b_a", 1), ("lru_b_x", 1), ("lru_lambda", 1),
                  ("conf_dw_w", 31), ("conf_dw_b", 1), ("conf_ln_g", 1), ("conf_ln_b", 1), ("conf_pw_b", 1)]
        self.ROW = {}
        r0 = 0
        for n, k in rowsrc:
            src = W[n] if k > 1 else W[n].rearrange("(o c) -> o c", o=1)
            self.dma("sp", self.rows[r0:r0 + k, :], src, [], [self.B_rows])
            self.ROW[n] = r0
            r0 += k
        self.dma("sp", self.rows[r0:r0 + 1, :], W["branch_norm_g"][512:1024].rearrange("(o c) -> o c", o=1), [], [self.B_rows])
        self.ROW["bg_lru"] = r0
        r0 += 1
        self.dma("sp", self.rows[r0:r0 + 1, :], W["branch_norm_g"][1536:2048].rearrange("(o c) -> o c", o=1), [], [self.B_rows])
        self.ROW["bg_conf"] = r0
        r0 += 1
        NR = r0
        for c in range(4):
            self.tr(self.ps[7][:, c * 48:c * 48 + NR], self.rows[0:NR, c * 128:(c + 1) * 128], self.identf[0:NR, 0:NR],
                    [self.B_rows, self.B_c], [self.B_ps[7]])
        for c in range(4):
            self.cp("dve", self.colv[:, c, 0:NR], self.ps[7][:, c * 48:c * 48 + NR], [self.B_ps[7]], [self.B_colv])

        m0 = self.off
        if l == 0:
            self.prefetch(W["w_in"][:, 0:512], 512, (0, 0))
        fused_in = (l > 0) and self.upto >= 5
        xt = [A([D], F32), A([D], F32)]
        hn = [A([D], BF16), A([D], BF16)]
        junk = A([D], BF16)
        st1 = A([NT, 2], F32)
        B_xt = [Buf(), Buf()]
        B_hn = [Buf(), Buf()]
        B_junk = Buf()
        B_st = [Buf() for _ in range(NT)]
        for t in range(NT if not fused_in else 0):
            i = t % 2
            self.dma("sp", xt[i], src_d[t * 128:(t + 1) * 128, :], [self.B_out[t]], [B_xt[i]])
            ss = st1[:, t, 0:1]
            self.act(junk, xt[i], AF.Square, [B_xt[i]], [B_junk, B_st[t]], accum_out=ss)
            self.ts("dve", ss, ss, 1.0 / D, 1e-6, ALU.mult, ALU.add, [B_st[t]], [B_st[t]])
            self.rsqrt(ss, B_st[t])
            self.stt("dve", hn[i], xt[i], ss, self.gvec, ALU.mult, ALU.mult, [B_xt[i], B_st[t], self.B_g], [B_hn[i]])
            pA = self.ps[2 * i].bitcast(BF16)
            pB = self.ps[2 * i + 1].bitcast(BF16)
            for kc in range(16):
                dst = (pA if kc < 8 else pB)[:, (kc % 8) * 128:(kc % 8 + 1) * 128]
                self.tr(dst, hn[i][:, kc * 128:(kc + 1) * 128], self.ident, [B_hn[i], self.B_c],
                        [self.B_ps[2 * i + (0 if kc < 8 else 1)]])
            self.cp("act", self.hT[:, 0:8, t * 128:(t + 1) * 128], pA.rearrange("p (a b) -> p a b", a=8),
                    [self.B_ps[2 * i]], [self.B_hT[t]])
            self.cp("dve", self.hT[:, 8:16, t * 128:(t + 1) * 128], pB.rearrange("p (a b) -> p a b", a=8),
                    [self.B_ps[2 * i + 1]], [self.B_hT[t]])
        P.barrier()
        self.off = m0
        if self.upto >= 1:
            self.gla(l)
            P.barrier()
            self.off = m0
        if self.upto >= 2:
            self.lru(l)
            P.barrier()
            self.off = m0
        if self.upto >= 3:
            self.nsa(l)
            P.barrier()
            self.off = m0
        if self.upto >= 4:
            self.conf(l)
            P.barrier()
            self.off = m0
        if self.upto >= 5:
            self.outproj(l, src_d)
            P.barrier()
            self.off = m0

    def tm_tail_alloc(self):
        A = self.alloc
        self.tl = dict(sq=A([512], F32), st=A([2, 4], F32), gz=A([2, 512], F32), mx=A([2, 512], BF16), ms=A([2, 512], BF16))
        self.B_tl = dict(sq=Buf(), st=[Buf(), Buf()], gz=[Buf(), Buf()], mx=[Buf(), Buf()], ms=[Buf(), Buf()])

    def tm_tail(self, o_sb, B_o, z_ap, B_z, gcol0, mixer, t, pbank):
        self.tm_tail_a(o_sb, B_o, z_ap, B_z, gcol0, t)
        self.tm_tail_b(mixer, t, pbank)

    def tm_tail_a(self, o_sb, B_o, z_ap, B_z, gcol0, t):
        self.tm_tail_a1(o_sb, B_o, t)
        self.tm_tail_a2(t)
        self.tm_tail_a3(o_sb, B_o, z_ap, B_z, gcol0, t)

    def tm_tail_a1(self, o_sb, B_o, t):
        i = t % 2
        tl, Bt = self.tl, self.B_tl
        self.act(tl["sq"], o_sb, AF.Square, [B_o], [Bt["sq"]])
        st = tl["st"][:, i, :]
        self.P.op("dve", lambda e: e.tensor_reduce(out=st, in_=tl["sq"].rearrange("p (h e) -> p h e", h=4), axis=AX.X, op=ALU.add),
                  reads=[Bt["sq"]], writes=[Bt["st"][i]])
        self.ts("dve", st, st, 1.0 / 128, 1e-6, ALU.mult, ALU.add, [Bt["st"][i]], [Bt["st"][i]])

    def tm_tail_a2(self, t):
        i = t % 2
        self.rsqrt(self.tl["st"][:, i, :], self.B_tl["st"][i])

    def tm_tail_a3(self, o_sb, B_o, z_ap, B_z, gcol0, t):
        i = t % 2
        tl, Bt = self.tl, self.B_tl
        st = tl["st"][:, i, :]
        gz = tl["gz"][:, i, :]
        self.tt("pool", gz, z_ap, self.bgbc[:, gcol0:gcol0 + 512], ALU.mult, [B_z, self.B_bg], [Bt["gz"][i]])
        mx = tl["mx"][:, i, :]
        self.tt("dve", gz.rearrange("p (h e) -> p h e", h=4), gz.rearrange("p (h e) -> p h e", h=4),
                st.unsqueeze(2).to_broadcast([128, 4, 128]), ALU.mult, [Bt["st"][i], Bt["gz"][i]], [Bt["gz"][i]])
        self.tt("dve", mx, o_sb, gz, ALU.mult, [B_o, Bt["gz"][i]], [Bt["mx"][i]])

    def tm_tail_b(self, mixer, t, pbank):
        i = t % 2
        tl, Bt = self.tl, self.B_tl
        mx = tl["mx"][:, i, :]
        pT = self.ps[pbank].bitcast(BF16)
        for h in range(4):
            self.tr(pT[:, h * 128:(h + 1) * 128], mx[:, h * 128:(h + 1) * 128], self.ident, [Bt["mx"][i], self.B_c], [self.B_ps[pbank]])
        ms = tl["ms"][:, i, :]
        self.evac(ms, pT[:, 0:512], [self.B_ps[pbank]], [Bt["ms"][i]])
        dst = self.mixT_d[mixer * 512:(mixer + 1) * 512, t * 128:(t + 1) * 128].rearrange("(c p) j -> p c j", p=128)
        self.dma("sp", dst, ms.rearrange("p (c j) -> p c j", c=4), [Bt["ms"][i]], [self.B_mixd[mixer]])

    def gla(self, l):
        A = self.alloc
        W = self.W
        win = W["w_in"]
        qT = A([2, S], BF16)
        kT = A([2, S], BF16)
        ktm = A([NT, 256], BF16)
        vtm = A([NT, 512], BF16)
        ztm = A([NT, 512], BF16)
        fgT = A([S], BF16)
        w2a = A([256], BF16)
        B_q, B_k, B_ktm, B_v, B_z, B_fg, B_w2 = Buf(), Buf(), [Buf() for _ in range(NT)], [Buf() for _ in range(NT)], [Buf() for _ in range(NT)], Buf(), Buf()
        self.memset("pool", fgT[0:32, :], 1.0, [B_fg])
        self.dma("pool", w2a[0:16, :], W["gla_w_fg2"], [], [B_w2])
        self.dma("pool", w2a[16:17, :], W["gla_b_fg2"].rearrange("(o c) -> o c", o=1), [], [B_w2])
        wap, wb = self.load_w(win[:, 0:512], 512, key=(l, 0))
        for g in range(2):
            self.proj_fm(wap, wb, g * 128, 128, lambda tb, g=g: (qT[:, g, tb * 512:(tb + 1) * 512], B_q), 0 if g == 0 else 4, func=AF.Copy, scale=0.125)
        for g in range(2):
            self.proj_fm(wap, wb, 256 + g * 128, 128, lambda tb, g=g: (kT[:, g, tb * 512:(tb + 1) * 512], B_k), 0 if g == 0 else 4)
        for t in range(NT):
            bk = t % 2
            pT = self.ps[bk].bitcast(BF16)
            for dc in range(2):
                self.tr(pT[:, dc * 128:(dc + 1) * 128], kT[:, dc, t * 128:(t + 1) * 128], self.ident, [B_k, self.B_c], [self.B_ps[bk]])
            self.evac(ktm[:, t, :], pT[:, 0:256], [self.B_ps[bk]], [B_ktm[t]])
        wap, wb = self.load_w(win[:, 512:1024], 512)
        self.proj_tm(wap, wb, 0, 512, lambda t: (vtm[:, t, :], B_v[t]))
        wap, wb = self.load_w(win[:, 1024:1040], 16)
        self.proj_fm(wap, wb, 0, 16, lambda tb: (fgT[0:16, tb * 512:(tb + 1) * 512], B_fg), 4)
        wap, wb = self.load_w(win[:, 1040:1552], 512)
        self.proj_tm(wap, wb, 0, 512, lambda t: (ztm[:, t, :], B_z[t]), func=AF.Silu)
        self.prefetch(win[:, 1552:2064], 512, (l, 1552))
        Lf = A([2, 256], F32)
        eq = A([2, 256], F32)
        ek = A([2, 256], F32)
        ekk = A([2, 256], F32)
        qd = A([2, 4, 128], BF16)
        kd = A([2, 256], BF16)
        kk = A([2, 256], BF16)
        at = A([2, 512], BF16)
        Sf = A([2, 128], F32)
        Sb = A([2, 128], BF16)
        osb = A([2, 512], F32)
        dec = A([2, 2], F32)
        B_L, B_eq, B_ek, B_ekk, B_qd, B_kd, B_kk, B_at, B_os, B_dec = ([Buf(), Buf()] for _ in range(10))
        B_Sf, B_Sb = Buf(), Buf()
        self.tm_tail_alloc()
        self.memset("pool", Sf, 0.0, [B_Sf])
        self.memset("pool", Sb, 0.0, [B_Sb])
        self.memset("pool", qd, 0.0, B_qd)
        ps, Bp = self.ps, self.B_ps
        def front(t):
                i = t % 2
                tsl = slice(t * 128, (t + 1) * 128)
                self.mm(ps[0][:, 0:256], fgT[0:17, tsl], w2a[0:17, :], True, True, [B_fg, B_w2], [Bp[0]])
                self.act(Lf[:, i, :], ps[0][:, 0:256], AF.Exp, [Bp[0]], [B_L[i]], scale=-1.0)
                self.act(Lf[:, i, :], Lf[:, i, :], AF.Ln, [B_L[i]], [B_L[i]], bias=1.0)
                for dc in range(2):
                    self.mm(ps[1][:, dc * 128:(dc + 1) * 128], Lf[:, i, dc * 128:(dc + 1) * 128], self.triI, True, True, [B_L[i], self.B_c], [Bp[1]])
                self.act(eq[:, i, :], ps[1][:, 0:256], AF.Exp, [Bp[1]], [B_eq[i]], scale=-1.0 / 16)
                self.act(ek[:, i, :], ps[1][:, 0:256], AF.Exp, [Bp[1]], [B_ek[i]], scale=1.0 / 16)
                self.mm(ps[2][:, 0:256], self.triS, Lf[:, i, :], True, True, [B_L[i], self.B_c], [Bp[2]])
                self.act(ekk[:, i, :], ps[2][:, 0:256], AF.Exp, [Bp[2]], [B_ekk[i]], scale=-1.0 / 16)
                for h in range(4):
                    dc, pb = h // 2, (h % 2) * 64
                    self.tt("dve", qd[pb:pb + 64, i, h, :], qT[pb:pb + 64, dc, tsl], eq[pb:pb + 64, i, dc * 128:(dc + 1) * 128], ALU.mult, [B_q, B_eq[i]], [B_qd[i]])
                for dc in range(2):
                    self.tt("pool", kd[:, i, dc * 128:(dc + 1) * 128], kT[:, dc, tsl], ek[:, i, dc * 128:(dc + 1) * 128], ALU.mult, [B_k, B_ek[i]], [B_kd[i]])
                self.tt("pool", kk[:, i, :], ktm[:, t, :], ekk[:, i, :], ALU.mult, [B_ktm[t], B_ekk[i]], [B_kk[i]])
                self.cp("dve", dec[:, i, :], eq[:, i, :].rearrange("p (a b) -> p a b", a=2)[:, :, 127], [B_eq[i]], [B_dec[i]])

        def back(t):
                i = t % 2
                tsl = slice(t * 128, (t + 1) * 128)
                for h in range(4):
                    dc, pb = h // 2, (h % 2) * 64
                    self.mm(ps[3][:, h * 128:(h + 1) * 128], kd[:, i, dc * 128:(dc + 1) * 128], qd[:, i, h, :],
                            h == 0, h == 3, [B_kd[i], B_qd[i]], [Bp[3]], skip_group_check=True)
                self.tt("dve", at[:, i, :], ps[3], self.mask4.rearrange("p a b -> p (a b)"), ALU.mult, [Bp[3], self.B_c], [B_at[i]])
                ob = 4 + i
                for h in range(4):
                    dc, pb = h // 2, (h % 2) * 64
                    self.mm(ps[ob][:, h * 128:(h + 1) * 128], at[:, i, h * 128:(h + 1) * 128], vtm[:, t, h * 128:(h + 1) * 128],
                            h == 0, False, [B_at[i], B_v[t]], [Bp[ob]], skip_group_check=True)
                    self.mm(ps[ob][:, h * 128:(h + 1) * 128], qd[:, i, h, :], Sb[:, dc, :],
                            False, h == 3, [B_qd[i], B_Sb], [Bp[ob]], skip_group_check=True)
                if t >= 2:
                    self.tm_tail_b(0, t - 2, 7)
                self.cp("act", osb[:, i, :], ps[ob], [Bp[ob]], [B_os[i]])
                if t < NT - 1:
                    for h in range(4):
                        dc = h // 2
                        self.mm(ps[6][:, h * 128:(h + 1) * 128], kk[:, i, dc * 128:(dc + 1) * 128], vtm[:, t, h * 128:(h + 1) * 128],
                                h == 0, h == 3, [B_kk[i], B_v[t]], [Bp[6]], skip_group_check=True)
                    for h in range(4):
                        dc, pb = h // 2, (h % 2) * 64
                        self.stt("dve", Sf[pb:pb + 64, dc, :], Sf[pb:pb + 64, dc, :], dec[pb:pb + 64, i, dc:dc + 1], ps[6][pb:pb + 64, h * 128:(h + 1) * 128], ALU.mult, ALU.add,
                                 [B_Sf, B_dec[i], Bp[6]], [B_Sf])
                    self.cp("dve", Sb, Sf, [B_Sf], [B_Sb])

        for t in range(NT + 3):
            if t < NT:
                front(t)
            if 3 <= t:
                u = t - 3
                self.tm_tail_a2(u)
                self.tm_tail_a3(osb[:, u % 2, :], B_os[u % 2], ztm[:, u, :], B_z[u], 0, u)
            if 1 <= t <= NT:
                back(t - 1)
            if 2 <= t <= NT + 1:
                u = t - 2
                self.tm_tail_a1(osb[:, u % 2, :], B_os[u % 2], u)
        self.tm_tail_b(0, NT - 2, 7)
        self.tm_tail_b(0, NT - 1, 7)

    def fm_tail_alloc(self):
        A = self.alloc
        self.ft = dict(hsq=A([2, 512], BF16), rs=A([2, 512], F32), mo=A([2, 512], BF16))
        self.B_ft = dict(hsq=[Buf(), Buf()], rs=[Buf(), Buf()], mo=[Buf(), Buf()])

    def fm_tail(self, h_ap, B_h, z_ap, B_z, gcol, row0, tb, i, pbank):
        self.fm_tail_pre(h_ap, B_h, i)
        self.fm_tail_post(h_ap, B_h, z_ap, B_z, gcol, row0, tb, i, pbank)

    def fm_tail_pre(self, h_ap, B_h, i):
        ft, Bf = self.ft, self.B_ft
        self.act(ft["hsq"][:, i, :], h_ap, AF.Square, [B_h], [Bf["hsq"][i]])

    def fm_tail_post(self, h_ap, B_h, z_ap, B_z, gcol, row0, tb, i, pbank):
        ft, Bf = self.ft, self.B_ft
        self.mm(self.ps[pbank], self.ones, ft["hsq"][:, i, :], True, True, [Bf["hsq"][i], self.B_c], [self.B_ps[pbank]])
        rs = ft["rs"][:, i, :]
        self.ts("dve", rs, self.ps[pbank], 1.0 / 128, 1e-6, ALU.mult, ALU.add, [self.B_ps[pbank]], [Bf["rs"][i]])
        self.rsqrt(rs, Bf["rs"][i])
        self.tt("dve", rs, h_ap, rs, ALU.mult, [B_h, Bf["rs"][i]], [Bf["rs"][i]])
        self.stt("dve", ft["mo"][:, i, :], rs, gcol, z_ap, ALU.mult, ALU.mult, [Bf["rs"][i], self.B_colv, B_z], [Bf["mo"][i]])
        self.dma("sp", self.mixT_d[row0:row0 + 128, tb * 512:(tb + 1) * 512], ft["mo"][:, i, :], [Bf["mo"][i]], [self.B_mixd[row0 // 512]])

    def lru(self, l):
        A = self.alloc
        W = self.W
        win = W["w_in"]
        ps, Bp = self.ps, self.B_ps
        R = self.ROW
        cv = self.colv
        xp = A([4, S + 4], BF16)
        zT = A([4, S], BF16)
        wa = A([4, 128], BF16)
        wx = A([4, 128], BF16)
        c8 = A([4, 2], F32)
        B_xp, B_zT, B_wa, B_c8 = Buf(), Buf(), Buf(), Buf()
        self.memset("pool", xp[:, :, 0:4], 0.0, [B_xp])
        self.dma("pool", wa, W["lru_w_a"].rearrange("h i j -> i h j"), [], [B_wa])
        self.dma("pool", wx, W["lru_w_x"].rearrange("h i j -> i h j"), [], [B_wa])
        wap, wb = self.load_w(win[:, 1552:2064], 512, key=(l, 1552))
        for g in range(4):
            self.proj_fm(wap, wb, g * 128, 128, lambda tb, g=g: (xp[:, g, 4 + tb * 512:4 + (tb + 1) * 512], B_xp), 0 if g % 2 == 0 else 4)
        wap, wb = self.load_w(win[:, 2064:2576], 512)
        for g in range(4):
            self.proj_fm(wap, wb, g * 128, 128, lambda tb, g=g: (zT[:, g, tb * 512:(tb + 1) * 512], B_zT), 0 if g % 2 == 0 else 4, func=AF.Silu)
        lam = cv[:, :, R["lru_lambda"]]
        self.act(c8[:, :, 0], lam, AF.Exp, [self.B_colv], [B_c8], scale=-1.0)
        self.act(c8[:, :, 0], c8[:, :, 0], AF.Ln, [B_c8], [B_c8], bias=1.0)
        self.ts("dve", c8[:, :, 1], c8[:, :, 0], -16.0, None, ALU.mult, None, [B_c8], [B_c8])
        self.ts("dve", c8[:, :, 0], c8[:, :, 0], -8.0, None, ALU.mult, None, [B_c8], [B_c8])
        self.prefetch(win[:, 2576:3088], 512, (l, 2576))
        xcs = [A([S], F32), A([S], F32)]
        rA = A([S], F32)
        ig = A([S], F32)
        a2 = A([S], F32)
        hb = A([S], F32)
        xcb = A([S], BF16)
        mo = A([S], BF16)
        B_xcs = [Buf(), Buf()]
        B_r, B_ig, B_a2, B_h, B_xcb, B_mo = (Buf() for _ in range(6))
        cw = R["lru_conv_w"]

        def conv_main(c):
            xc, Bx = xcs[c % 2], B_xcs[c % 2]
            self.act(xc, xp[:, c, 1:1 + S], AF.Identity, [B_xp, self.B_colv], [Bx],
                     scale=cv[:, c, cw:cw + 1], bias=cv[:, c, R["lru_conv_b"]:R["lru_conv_b"] + 1])
            for k in range(1, 4):
                self.stt("dve", xc, xp[:, c, 1 + k:1 + k + S], cv[:, c, cw + k:cw + k + 1], xc, ALU.mult, ALU.add,
                         [B_xp, self.B_colv, Bx], [Bx])

        def conv_cast(c):
            self.cp("act", xcb, xcs[c % 2], [B_xcs[c % 2]], [B_xcb])

        conv_main(0)
        conv_cast(0)
        for c in range(4):
            xc, Bx = xcs[c % 2], B_xcs[c % 2]
            for tb in range(4):
                self.mm(ps[tb], wa[:, c, :], xcb[:, tb * 512:(tb + 1) * 512], True, True, [B_wa, B_xcb], [Bp[tb]])
            for tb in range(4):
                self.mm(ps[4 + tb], wx[:, c, :], xcb[:, tb * 512:(tb + 1) * 512], True, True, [B_wa, B_xcb], [Bp[4 + tb]])
            for tb in range(4):
                self.act(rA[:, tb * 512:(tb + 1) * 512], ps[tb], AF.Sigmoid, [Bp[tb], self.B_colv], [B_r], bias=cv[:, c, R["lru_b_a"]:R["lru_b_a"] + 1])
            for tb in range(4):
                self.act(ig[:, tb * 512:(tb + 1) * 512], ps[4 + tb], AF.Sigmoid, [Bp[4 + tb], self.B_colv], [B_ig], bias=cv[:, c, R["lru_b_x"]:R["lru_b_x"] + 1])
            self.act(a2, rA, AF.Exp, [B_r, B_c8], [B_a2], scale=c8[:, c, 1:2])
            self.act(rA, rA, AF.Exp, [B_r, B_c8], [B_r], scale=c8[:, c, 0:1])
            self.act(a2, a2, AF.Sqrt, [B_a2], [B_a2], scale=-1.0, bias=1.0)
            self.tt("dve", ig, ig, xc, ALU.mult, [B_ig, Bx], [B_ig])
            self.tt("dve", ig, ig, a2, ALU.mult, [B_ig, B_a2], [B_ig])
            self.P.op("dve", lambda e: e.tensor_tensor_scan(out=hb, data0=rA, data1=ig, initial=0.0, op0=ALU.mult, op1=ALU.add),
                      reads=[B_r, B_ig], writes=[B_h])
            if c + 1 < 4:
                conv_main(c + 1)
            self.act(mo, hb, AF.Square, [B_h], [B_mo])
            for tb in range(4):
                self.mm(ps[tb], self.ones, mo[:, tb * 512:(tb + 1) * 512], True, True, [B_mo, self.B_c], [Bp[tb]])
            for tb in range(4):
                self.act(a2[:, tb * 512:(tb + 1) * 512], ps[tb], AF.Ln, [Bp[tb]], [B_a2], scale=1.0 / 128, bias=1e-6)
            self.act(a2, a2, AF.Exp, [B_a2], [B_a2], scale=-0.5)
            self.tt("dve", a2, hb, a2, ALU.mult, [B_h, B_a2], [B_a2])
            self.stt("dve", mo, a2, cv[:, c, R["bg_lru"]:R["bg_lru"] + 1], zT[:, c, :], ALU.mult, ALU.mult, [B_a2, self.B_colv, B_zT], [B_mo])
            self.dma("sp", self.mixT_d[512 + c * 128:512 + (c + 1) * 128, :], mo, [B_mo], [self.B_mixd[1]])
            if c + 1 < 4:
                conv_cast(c + 1)

    def conf(self, l):
        A = self.alloc
        W = self.W
        win = W["w_in"]
        ps, Bp = self.ps, self.B_ps
        R = self.ROW
        cv = self.colv
        PADC = 32
        yp = A([4, S + PADC], BF16)
        zT = A([4, S], BF16)
        pw = A([4, 512], BF16)
        sg_off = self.off
        sg = A([4, S], BF16)
        B_yp, B_sg, B_zT, B_pw, B_dg = Buf(), Buf(), Buf(), Buf(), Buf()
        self.memset("pool", yp[:, :, 0:PADC], 0.0, [B_yp])
        self.dma("pool", pw, W["conf_pw_w"].rearrange("(k p) c -> p k c", p=128), [], [B_pw])
        wap, wb = self.load_w(win[:, 4380:4892], 512, key=(l, 4380))
        for g in range(4):
            self.proj_fm(wap, wb, g * 128, 128, lambda tb, g=g: (yp[:, g, PADC + tb * 512:PADC + (tb + 1) * 512], B_yp), 0 if g % 2 == 0 else 4)
        wap, wb = self.load_w(win[:, 4892:5404], 512)
        for g in range(4):
            self.proj_fm(wap, wb, g * 128, 128, lambda tb, g=g: (sg[:, g, tb * 512:(tb + 1) * 512], B_sg), 0 if g % 2 == 0 else 4, func=AF.Sigmoid)
        wap, wb = self.load_w(win[:, 5404:5916], 512)
        for g in range(4):
            self.proj_fm(wap, wb, g * 128, 128, lambda tb, g=g: (zT[:, g, tb * 512:(tb + 1) * 512], B_zT), 0 if g % 2 == 0 else 4, func=AF.Silu)
        for g in range(4):
            self.tt("pool" if g % 2 else "dve", yp[:, g, PADC:PADC + S], yp[:, g, PADC:PADC + S], sg[:, g, :], ALU.mult, [B_yp, B_sg], [B_yp])
        self.P.barrier()
        dgs = [self.wbuf[0][:, :, :].rearrange("p a b -> p (a b)"), self.wbuf[1][:, :, :].rearrange("p a b -> p (a b)")]

        def dg(c, k):
            idx = c * 31 + k
            return dgs[idx // 62][:, (idx % 62) * 128:(idx % 62 + 1) * 128]
        for c in range(4):
            for k in range(31):
                sc_ = cv[:, c, R["conf_dw_w"] + k:R["conf_dw_w"] + k + 1]
                if (c * 31 + k) % 2 == 0:
                    self.ts("dve", dg(c, k), self.identf, sc_, None, ALU.mult, None,
                            [self.B_c, self.B_colv, self.B_w[0], self.B_w[1]], [B_dg])
                else:
                    self.act(dg(c, k), self.identf, AF.Copy, [self.B_c, self.B_colv, self.B_w[0], self.B_w[1]], [B_dg], scale=sc_)
        if self.upto >= 5:
            mview = self.mixT_d.rearrange("(k p) t -> p k t", p=128)
            self.dma("sp", self.hT[:, 0:12, :], mview[:, 0:12, :], self.B_mixd[0:3] + self.B_hT, self.B_hT)
            self.mT_pre = True
        self.off = sg_off
        cvf = A([2, 4, 512], F32)
        cvb = A([2, 512], BF16)
        sqb = A([2, 512], BF16)
        mean = A([2, 512], F32)
        rstd = A([2, 512], F32)
        tmp = A([2, 512], F32)
        sT = A([4, 512], BF16)
        od = A([2, 512], F32)
        B_cvf = [[Buf() for _ in range(4)] for _ in range(2)]
        B_cvb, B_sqb, B_tmp, B_od, B_mean, B_rstd = ([Buf(), Buf()] for _ in range(6))
        B_sT = [Buf() for _ in range(4)]
        self.fm_tail_alloc()
        self.cn = 0

        def convstage(tb, cs=(0, 1, 2, 3)):
            j = tb % 2
            sb0 = 2 + 2 * j
            for c in cs:
                i = self.cn % 2
                self.cn += 1
                for k in range(31):
                    o0 = PADC - 30 + k + tb * 512
                    self.mm(ps[i], dg(c, k), yp[:, c, o0:o0 + 512], k == 0, k == 30, [B_dg, B_yp], [Bp[i]])
                self.act(cvf[:, j, c, :], ps[i], AF.Identity, [Bp[i], self.B_colv], [B_cvf[j][c]], bias=cv[:, c, R["conf_dw_b"]:R["conf_dw_b"] + 1])
                self.act(cvb[:, i, :], cvf[:, j, c, :], AF.Copy, [B_cvf[j][c]], [B_cvb[i]])
                self.act(sqb[:, i, :], cvf[:, j, c, :], AF.Square, [B_cvf[j][c]], [B_sqb[i]])
                self.mm(ps[sb0], self.ones, cvb[:, i, :], c == 0, c == 3, [self.B_c, B_cvb[i]], [Bp[sb0]])
                self.mm(ps[sb0 + 1], self.ones, sqb[:, i, :], c == 0, c == 3, [self.B_c, B_sqb[i]], [Bp[sb0 + 1]])

        def lnstage(tb):
            j = tb % 2
            sb0 = 2 + 2 * j
            mn, rs_ = mean[:, j, :], rstd[:, j, :]
            self.ts("dve", mn, ps[sb0], 1.0 / 512, None, ALU.mult, None, [Bp[sb0]], [B_mean[j]])
            self.ts("dve", rs_, ps[sb0 + 1], 1.0 / 512, 1e-6, ALU.mult, ALU.add, [Bp[sb0 + 1]], [B_rstd[j]])
            self.tt("dve", tmp[:, 0, :], mn, mn, ALU.mult, [B_mean[j]], [B_tmp[0]])
            self.tt("dve", rs_, rs_, tmp[:, 0, :], ALU.subtract, [B_rstd[j], B_tmp[0]], [B_rstd[j]])
            self.rsqrt(rs_, B_rstd[j])
            for c in range(4):
                i = c % 2
                self.tt("dve", tmp[:, i, :], cvf[:, j, c, :], mn, ALU.subtract, [B_cvf[j][c], B_mean[j]], [B_tmp[i]])
                self.tt("dve", tmp[:, i, :], tmp[:, i, :], rs_, ALU.mult, [B_tmp[i], B_rstd[j]], [B_tmp[i]])
                self.act(sT[:, c, :], tmp[:, i, :], AF.Silu, [B_tmp[i], self.B_colv], [B_sT[c]],
                         scale=cv[:, c, R["conf_ln_g"]:R["conf_ln_g"] + 1], bias=cv[:, c, R["conf_ln_b"]:R["conf_ln_b"] + 1])

        def pwstage(tb, jc):
            i = jc % 2
            for ic in range(4):
                self.mm(ps[6], pw[:, ic, jc * 128:(jc + 1) * 128], sT[:, ic, :], ic == 0, ic == 3, [B_pw, B_sT[ic]], [Bp[6]])
            self.act(od[:, i, :], ps[6], AF.Identity, [Bp[6], self.B_colv], [B_od[i]], bias=cv[:, jc, R["conf_pw_b"]:R["conf_pw_b"] + 1])
            self.fm_tail_pre(od[:, i, :], B_od[i], i)

        def tlstage(tb, jc):
            i = jc % 2
            self.fm_tail_post(od[:, i, :], B_od[i], zT[:, jc, tb * 512:(tb + 1) * 512], B_zT, cv[:, jc, R["bg_conf"]:R["bg_conf"] + 1], 1536 + jc * 128, tb, i, 7)

        convstage(0)
        for tb in range(4):
            nx = tb + 1 < 4
            lnstage(tb)
            if nx:
                convstage(tb + 1, (0, 1))
            pwstage(tb, 0)
            if nx:
                convstage(tb + 1, (2,))
            tlstage(tb, 0)
            pwstage(tb, 1)
            if nx:
                convstage(tb + 1, (3,))
            tlstage(tb, 1)
            pwstage(tb, 2)
            tlstage(tb, 2)
            pwstage(tb, 3)
            tlstage(tb, 3)

    def nsa(self, l):
        A = self.alloc
        W = self.W
        win = W["w_in"]
        ps, Bp = self.ps, self.B_ps
        qT = A([4, S], BF16)
        kcT = A([S], BF16)
        vcT = A([S], BF16)
        ksT = A([S], BF16)
        kwT = A([S], BF16)
        vs = A([NT, 130], BF16)
        vw = A([NT, 130], BF16)
        ztm = A([NT, 512], BF16)
        gt = A([NT, 12], F32)
        EX = A([S], BF16)
        kcmpT = A([128], BF16)
        vaug = A([162], BF16)
        mlp_off = self.off
        posr = A([128], F32)
        posT = A([64], BF16)
        w2k = A([128], BF16)
        w2v = A([128], BF16)
        hid = A([2, 128], BF16)
        cb = A([2], F32)
        B_q, B_kc, B_vc, B_ks, B_kw, B_EX, B_pos, B_w2, B_kcmp, B_vaug, B_hid, B_cb = (Buf() for _ in range(12))
        B_vs = [Buf() for _ in range(NT)]
        B_vw = [Buf() for _ in range(NT)]
        B_z = [Buf() for _ in range(NT)]
        B_gt = [Buf() for _ in range(NT)]
        self.memset("pool", vs, 1.0, B_vs)
        self.memset("pool", vw, 1.0, B_vw)
        self.memset("pool", EX, BIG, [B_EX])
        self.asel(EX, EX, [[1, S]], ALU.is_ge, 0.0, 0, -64, [B_EX], [B_EX])
        self.asel(EX, EX, [[-1, S]], ALU.is_ge, 0.0, 63, 64, [B_EX], [B_EX])
        self.memset("pool", vaug, 1.0, [B_vaug])
        self.asel(vaug[:, 130:162], vaug[:, 130:162], [[-4, 32]], ALU.is_ge, 0.0, 1, 1, [B_vaug], [B_vaug])
        self.asel(vaug[:, 130:162], vaug[:, 130:162], [[4, 32]], ALU.is_ge, 0.0, 3, -1, [B_vaug], [B_vaug])
        self.dma("sp", posr[0:32, :], W["nsa_cmp_pos_k"], [], [B_pos])
        self.dma("sp", posr[32:64, :], W["nsa_cmp_pos_v"], [], [B_pos])
        self.dma("pool", w2k, W["nsa_cmp_w2_k"], [], [B_w2])
        self.dma("pool", w2v, W["nsa_cmp_w2_v"], [], [B_w2])
        wap, wb = self.load_w(win[:, 2576:3088], 512, key=(l, 2576))
        for g in range(4):
            self.proj_fm(wap, wb, g * 128, 128, lambda tb, g=g: (qT[:, g, tb * 512:(tb + 1) * 512], B_q), 0 if g % 2 == 0 else 4, func=AF.Copy, scale=128.0 ** -0.5)
        wap, wb = self.load_w(win[:, 3088:3600], 512)
        self.proj_fm(wap, wb, 0, 128, lambda tb: (kcT[:, tb * 512:(tb + 1) * 512], B_kc), 0)
        self.proj_fm(wap, wb, 128, 128, lambda tb: (vcT[:, tb * 512:(tb + 1) * 512], B_vc), 4)
        self.proj_fm(wap, wb, 256, 128, lambda tb: (ksT[:, tb * 512:(tb + 1) * 512], B_ks), 0)
        self.proj_tm(wap, wb, 384, 128, lambda t: (vs[:, t, 0:128], B_vs[t]))
        wap, wb = self.load_w(win[:, 3600:3868], 268)
        self.proj_fm(wap, wb, 0, 128, lambda tb: (kwT[:, tb * 512:(tb + 1) * 512], B_kw), 4)
        self.proj_tm(wap, wb, 128, 128, lambda t: (vw[:, t, 0:128], B_vw[t]))
        self.proj_tm(wap, wb, 256, 12, lambda t: (gt[:, t, :], B_gt[t]), func=AF.Sigmoid)
        wap, wb = self.load_w(win[:, 3868:4380], 512)
        self.proj_tm(wap, wb, 0, 512, lambda t: (ztm[:, t, :], B_z[t]), func=AF.Silu)
        self.P.barrier()
        w1 = [self.wbuf[0][:, :, :].rearrange("p a b -> p (a b)").rearrange("p (a b) -> p a b", a=64),
              None]
        w1k = w1[0][:, 0:32, :]
        w1v = w1[0][:, 32:64, :]
        self.dma("pool", w1k, W["nsa_cmp_w1_k"].rearrange("(p d) h -> d p h", d=128), [], [self.B_w[0]])
        self.dma("pool", w1v, W["nsa_cmp_w1_v"].rearrange("(p d) h -> d p h", d=128), [], [self.B_w[0]])
        self.tr(ps[0][:, 0:64], posr[0:64, :], self.identf[0:64, 0:64], [B_pos, self.B_c], [Bp[0]])
        self.cp("dve", posT, ps[0][:, 0:64], [Bp[0]], [B_pos])
        for kv in range(2):
            w1x = w1k if kv == 0 else w1v
            src = kcT if kv == 0 else vcT
            Bs = B_kc if kv == 0 else B_vc
            for p in range(32):
                self.mm(ps[1][:, 0:1], w1x[:, p, :], posT[:, kv * 32 + p:kv * 32 + p + 1], p == 0, p == 31, [self.B_w[0], B_pos], [Bp[1]])
            self.cp("dve", cb[:, kv:kv + 1], ps[1][:, 0:1], [Bp[1]], [B_cb])
            for p in range(32):
                self.mm(ps[2][:, 0:127], w1x[:, p, :], src[:, p:p + 16 * 126 + 1:16], p == 0, p == 31, [self.B_w[0], Bs], [Bp[2]])
            self.act(hid[:, kv, 0:127], ps[2][:, 0:127], AF.Silu, [Bp[2], B_cb], [B_hid], bias=cb[:, kv:kv + 1])
        self.mm(ps[3][:, 0:127], w2k, hid[:, 0, 0:127], True, True, [B_w2, B_hid], [Bp[3]])
        self.cp("dve", kcmpT[:, 0:127], ps[3][:, 0:127], [Bp[3]], [B_kcmp])
        self.mm(ps[4][0:127, 0:128], hid[:, 1, 0:127], w2v, True, True, [B_w2, B_hid], [Bp[4]])
        self.cp("dve", vaug[0:127, 0:128], ps[4][0:127, 0:128], [Bp[4]], [B_vaug])
        self.P.barrier()
        self.wslot = 1
        self.prefetch(win[:, 4380:4892], 512, (l, 4380))
        self.off = mlp_off
        pS = A([2, 512], BF16)
        pC = A([2, 512], BF16)
        imp = A([32], F32)
        imp2 = A([32], F32)
        m8 = A([16], F32)
        nm = A([32], BF16)
        nmT = A([2, 4, 128], BF16)
        cfc = A([8], F32)
        cf = A([8], F32)
        ocs = A([2, 512], F32)
        osb = A([2, 512], F32)
        B_pS, B_pC, B_os, B_ocs, B_nmT = ([Buf(), Buf()] for _ in range(5))
        B_imp, B_imp2, B_m8, B_nm, B_cfc, B_cf = (Buf() for _ in range(6))
        self.tm_tail_alloc()
        self.memset("pool", nmT, 0.0, B_nmT)
        self.nsc = 0
        M1 = A([8, 32], F32)
        M2 = A([8, 32], F32)
        B_M = Buf()
        self.memset("pool", M1, 1.0, [B_M])
        self.memset("pool", M2, 0.0, [B_M])
        for T in range(8, NT):
            for hf in range(2):
                cur = 2 * T + hf
                sl = slice(hf * 64, (hf + 1) * 64)
                if cur + 1 < 32:
                    self.memset("pool", M1[sl, T - 8, cur + 1:32], 0.0, [B_M])
                    self.memset("pool", M2[sl, T - 8, cur + 1:32], NEG, [B_M])
                self.memset("pool", M1[sl, T - 8, 0:1], 0.0, [B_M])
                self.memset("pool", M2[sl, T - 8, 0:1], 1000.0, [B_M])
                self.memset("pool", M1[sl, T - 8, cur - 1:cur + 1], 0.0, [B_M])
                self.memset("pool", M2[sl, T - 8, cur - 1:cur + 1], 1000.0, [B_M])

        def qt_(T):
            return qT[:, :, T * 128:(T + 1) * 128]

        def oc(h, a, b):
            return ps[2 + h // 2][:, (h % 2) * 162 + a:(h % 2) * 162 + b]

        def A1(T):
            p = T % 2
            sb = self.nsc % 2
            self.mm(ps[sb][0:127, :], kcmpT[:, 0:127], qt_(T), True, True, [B_kcmp, B_q], [Bp[sb]])
            self.act(pC[0:127, p, :], ps[sb][0:127, :], AF.Exp, [Bp[sb]], [B_pC[p]])
            v = pC[0:127, p, :].rearrange("p (h t) -> p h t", h=4)
            self.asel(v, v, [[0, 4], [1, 128]], ALU.is_ge, 0.0, 128 * T - 31, -16, [B_pC[p]], [B_pC[p]])

        def A2(T):
            p = T % 2
            for h in range(4):
                bk = 2 + h // 2
                c0 = (h % 2) * 162
                self.mm(ps[bk][:, c0:c0 + 162], pC[0:127, p, h * 128:(h + 1) * 128], vaug[0:127, 0:162], h % 2 == 0, h % 2 == 1,
                        [B_pC[p], B_vaug], [Bp[bk]], skip_group_check=True)

        def A3(T):
            p = T % 2
            for bk in range(2):
                self.ts("dve", cfc[:, 2 * bk:2 * bk + 2], ps[2 + bk][:, 128:128 + 2 * 162:162], 1e-30, None, ALU.max, None, [Bp[2 + bk]], [B_cfc])
            self.P.op("dve", lambda e: e.reciprocal(out=cfc[:, 0:4], in_=cfc[:, 0:4]), reads=[B_cfc], writes=[B_cfc])
            if T >= 8:
                self.ts("dve", imp, oc(0, 130, 162), cfc[:, 0:1], None, ALU.mult, None, [Bp[2], B_cfc], [B_imp])
                for h in range(1, 4):
                    self.stt("dve", imp, oc(h, 130, 162), cfc[:, h:h + 1], imp, ALU.mult, ALU.add, [Bp[2 + h // 2], B_cfc, B_imp], [B_imp])
                self.tt("dve", imp, imp, M1[:, T - 8, :], ALU.mult, [B_imp, B_M], [B_imp])
                self.tt("dve", imp, imp, M2[:, T - 8, :], ALU.add, [B_imp, B_M], [B_imp])
                self.P.op("dve", lambda e: e.max(out=m8[:, 0:8], in_=imp), reads=[B_imp], writes=[B_m8])
                self.P.op("dve", lambda e: e.match_replace(out=imp2, in_to_replace=m8[:, 0:8], in_values=imp, imm_value=-2.0e30), reads=[B_imp, B_m8], writes=[B_imp2])
                self.P.op("dve", lambda e: e.max(out=m8[:, 8:16], in_=imp2), reads=[B_imp2], writes=[B_m8])
                self.ts("dve", nm, imp, m8[:, 15:16], 1.0, ALU.is_ge, ALU.subtract, [B_imp, B_m8], [B_nm])
            self.tt("dve", cfc[:, 4:8], cfc[:, 0:4], gt[:, T, 0:12:3], ALU.mult, [B_cfc, B_gt[T]], [B_cfc])
            for bk in range(2):
                src = ps[2 + bk][:, 0:324].rearrange("p (a b) -> p a b", a=2)[:, :, 0:128]
                self.tt("dve", ocs[:, p, bk * 256:(bk + 1) * 256].rearrange("p (a b) -> p a b", a=2), src,
                        cfc[:, 4 + 2 * bk:6 + 2 * bk].unsqueeze(2).to_broadcast([128, 2, 128]), ALU.mult, [Bp[2 + bk], B_cfc], [B_ocs[p]])

        def A4(T):
            p = T % 2
            if T >= 8:
                sb = self.nsc % 2
                pT = ps[sb].bitcast(BF16)
                self.tr(pT[0:32, 0:128], nm, self.ident, [B_nm, self.B_c], [Bp[sb]])
                self.cp("dve", nmT[0:32, p, :, :], pT[0:32, 0:128].unsqueeze(1).to_broadcast([32, 4, 128]), [Bp[sb]], [B_nmT[p]])

        def score(T, job):
            kind, kt = job
            p = T % 2
            i = self.nsc % 2
            self.nsc += 1
            use_sel = T >= 8
            if kind == "slc":
                self.mm(ps[i], ksT[:, kt * 128:(kt + 1) * 128], qt_(T), True, not use_sel, [B_ks, B_q], [Bp[i]])
                if use_sel:
                    self.mm(ps[i], EX[:, kt * 128:(kt + 1) * 128], nmT[:, p, :, :], False, True, [B_EX, B_nmT[p]], [Bp[i]])
            else:
                self.mm(ps[i], kwT[:, kt * 128:(kt + 1) * 128], qt_(T), True, True, [B_kw, B_q], [Bp[i]])
            self.act(pS[:, i, :], ps[i], AF.Exp, [Bp[i]], [B_pS[i]])
            v = pS[:, i, :].rearrange("p (h t) -> p h t", h=4)
            if kt == T:
                self.asel(v, v, [[0, 4], [1, 128]], ALU.is_ge, 0.0, 0, -1, [B_pS[i]], [B_pS[i]])
            if kind == "win" and kt == T - 4:
                self.asel(v, v, [[0, 4], [-1, 128]], ALU.is_ge, 0.0, -1, 1, [B_pS[i]], [B_pS[i]])
            return i

        def pv(T, job, i, first, last):
            kind, kt = job
            base = 4 if kind == "slc" else 6
            vv, Bv = (vs, B_vs) if kind == "slc" else (vw, B_vw)
            for h in range(4):
                bk = base + h // 2
                c0 = (h % 2) * 130
                self.mm(ps[bk][:, c0:c0 + 129], pS[:, i, h * 128:(h + 1) * 128], vv[:, kt, 0:129], first and h % 2 == 0, last and h % 2 == 1,
                        [B_pS[i], Bv[kt]], [Bp[bk]], skip_group_check=True)

        acc = A([4, 260], F32)
        B_acc = Buf()

        def C(T):
            p = T % 2
            for bk in range(4):
                self.act(acc[:, bk, :], ps[4 + bk][:, 0:260], AF.Copy, [Bp[4 + bk]], [B_acc])
            for bk in range(4):
                self.ts("dve", cf[:, 2 * bk:2 * bk + 2], acc[:, bk, 128:260:130], 1e-30, None, ALU.max, None, [B_acc], [B_cf])
            self.P.op("dve", lambda e: e.reciprocal(out=cf, in_=cf), reads=[B_cf], writes=[B_cf])
            self.tt("dve", cf.rearrange("p (b h) -> p b h", b=2), cf.rearrange("p (b h) -> p b h", b=2),
                    gt[:, T, :].rearrange("p (h b) -> p b h", b=3)[:, 1:3, :], ALU.mult, [B_cf, B_gt[T]], [B_cf])
            for h in range(4):
                o_h = osb[:, p, h * 128:(h + 1) * 128]
                self.stt("dve", o_h, acc[:, h // 2, (h % 2) * 130:(h % 2) * 130 + 128], cf[:, h:h + 1], ocs[:, p, h * 128:(h + 1) * 128], ALU.mult, ALU.add,
                         [B_acc, B_cf, B_ocs[p]], [B_os[p]])
                self.stt("dve", o_h, acc[:, 2 + h // 2, (h % 2) * 130:(h % 2) * 130 + 128], cf[:, 4 + h:5 + h], o_h, ALU.mult, ALU.add,
                         [B_acc, B_cf, B_os[p]], [B_os[p]])


        A1(0)
        A2(0)
        A3(0)
        A4(0)
        for T in range(NT):
            jobs = [("slc", kt) for kt in range(T + 1)] + [("win", kt) for kt in range(max(0, T - 4), T + 1)]
            firsts = {("slc", 0), ("win", max(0, T - 4))}
            nxt = T + 1 < NT
            slot = score(T, jobs[0])
            for n, job in enumerate(jobs):
                nslot = score(T, jobs[n + 1]) if n + 1 < len(jobs) else None
                if nxt and n == 0:
                    A1(T + 1)
                pv(T, job, slot, job in firsts, job[1] == T)
                if nxt and n == min(1, len(jobs) - 1):
                    A2(T + 1)
                    A3(T + 1)
                if T >= 1 and n == min(1, len(jobs) - 1):
                    self.tm_tail_a1(osb[:, (T - 1) % 2, :], B_os[(T - 1) % 2], T - 1)
                if T >= 2 and n == min(1, len(jobs) - 1):
                    self.tm_tail_b(2, T - 2, self.nsc % 2)
                slot = nslot
            if T >= 1:
                u = T - 1
                self.tm_tail_a2(u)
                self.tm_tail_a3(osb[:, u % 2, :], B_os[u % 2], ztm[:, u, :], B_z[u], 512, u)
            if nxt:
                A4(T + 1)
            C(T)
        u = NT - 1
        self.tm_tail_a1(osb[:, u % 2, :], B_os[u % 2], u)
        self.tm_tail_a2(u)
        self.tm_tail_a3(osb[:, u % 2, :], B_os[u % 2], ztm[:, u, :], B_z[u], 512, u)
        self.tm_tail_b(2, NT - 2, 0)
        self.tm_tail_b(2, NT - 1, 1)

    def outproj(self, l, src_d):
        A = self.alloc
        W = self.W
        ps, Bp = self.ps, self.B_ps
        mT = self.hT
        fuse = (l + 1 < self.L) and self.upto >= 5
        mview = self.mixT_d.rearrange("(k p) t -> p k t", p=128)
        if not getattr(self, "mT_pre", False):
            self.dma("sp", mT[:, 0:12, :], mview[:, 0:12, :], self.B_mixd[0:3] + self.B_hT, self.B_hT)
        self.mT_pre = False
        self.dma("sp", mT[:, 12:16, :], mview[:, 12:16, :], [self.B_mixd[3]] + self.B_hT, self.B_hT)
        wo = A([16, D], BF16)
        B_wo = Buf()
        for cbk in range(4):
            self.dma("pool", wo[:, :, cbk * 512:(cbk + 1) * 512], W["w_out"][:, cbk * 512:(cbk + 1) * 512].rearrange("(k p) c -> p k c", p=128), [], [B_wo])
        self.dma("sp", self.gvec, W["post_norm_g"].partition_broadcast(128), [self.B_g], [self.B_g])
        if fuse:
            self.prefetch(self.w["w_in"][l + 1][:, 0:512], 512, (l + 1, 0))
            gpre = A([D], F32)
            B_gpre = Buf()
            self.dma("sp", gpre, self.w["pre_norm_g"][l + 1].partition_broadcast(128), [], [B_gpre])
        xt = A([D], F32)
        tmp = A([2, 512], F32)
        hn = A([D], BF16)
        st4 = A([NT, 4], F32)
        st = A([NT, 2], F32)
        B_xt, B_hn = Buf(), Buf()
        B_tmp = [Buf(), Buf()]
        B_st = [Buf() for _ in range(NT)]
        B_st4 = [Buf() for _ in range(NT)]
        def post(tp):
            bb = (tp % 2) * 4
            pA = ps[bb].bitcast(BF16)
            pB = ps[bb + 1].bitcast(BF16)
            for kc in range(16):
                dst = (pA if kc < 8 else pB)[:, (kc % 8) * 128:(kc % 8 + 1) * 128]
                self.tr(dst, hn[:, kc * 128:(kc + 1) * 128], self.ident, [B_hn, self.B_c], [Bp[bb + (0 if kc < 8 else 1)]])
            self.cp("act", self.hT[:, 0:8, tp * 128:(tp + 1) * 128], pA.rearrange("p (a b) -> p a b", a=8), [Bp[bb]], [self.B_hT[tp]])
            self.cp("dve", self.hT[:, 8:16, tp * 128:(tp + 1) * 128], pB.rearrange("p (a b) -> p a b", a=8), [Bp[bb + 1]], [self.B_hT[tp]])

        pend = None
        for t in range(NT):
            b0 = (t % 2) * 4
            self.dma("sp", xt, src_d[t * 128:(t + 1) * 128, :], [self.B_out[t]], [B_xt])
            for cbk in range(4):
                b = b0 + cbk
                for kc in range(16):
                    self.mm(ps[b], mT[:, kc, t * 128:(t + 1) * 128], wo[:, kc, cbk * 512:(cbk + 1) * 512], kc == 0, kc == 15, [self.B_hT[t], B_wo], [Bp[b]])
            if pend is not None:
                post(pend)
                pend = None
            for cbk in range(4):
                b = b0 + cbk
                self.act(hn[:, cbk * 512:(cbk + 1) * 512], ps[b], AF.Square, [Bp[b]], [B_hn, B_st4[t]], accum_out=st4[:, t, cbk:cbk + 1])
            ss = st[:, t, 0:1]
            self.P.op("dve", lambda e, ss=ss, t=t: e.tensor_reduce(out=ss, in_=st4[:, t, :], axis=AX.X, op=ALU.add), reads=[B_st4[t]], writes=[B_st[t]])
            self.ts("dve", ss, ss, 1.0 / D, 1e-6, ALU.mult, ALU.add, [B_st[t]], [B_st[t]])
            self.rsqrt(ss, B_st[t])
            for cbk in range(4):
                b = b0 + cbk
                j = cbk % 2
                cs = slice(cbk * 512, (cbk + 1) * 512)
                self.stt("dve", tmp[:, j, :], ps[b], ss, self.gvec[:, cs], ALU.mult, ALU.mult, [Bp[b], B_st[t], self.B_g], [B_tmp[j]])
                self.tt("pool" if j else "dve", xt[:, cs], xt[:, cs], tmp[:, j, :], ALU.add, [B_xt, B_tmp[j]], [B_xt])
            self.dma("sp", self.out_d[t * 128:(t + 1) * 128, :], xt, [B_xt], [self.B_out[t]])
            if fuse:
                s2 = st[:, t, 1:2]
                self.act(hn, xt, AF.Square, [B_xt], [B_hn, B_st[t]], accum_out=s2)
                self.ts("dve", s2, s2, 1.0 / D, 1e-6, ALU.mult, ALU.add, [B_st[t]], [B_st[t]])
                self.rsqrt(s2, B_st[t])
                self.stt("dve", hn, xt, s2, gpre, ALU.mult, ALU.mult, [B_xt, B_st[t], B_gpre], [B_hn])
                pend = t
        if pend is not None:
            post(pend)


def build_nc(L=4, upto=99, dbg=False):
    nc = bass.Bass("TRN2", target_bir_lowering=False)
    Builder(nc, L, upto, dbg).build()
    return nc


def kernel(**inputs):
    L = 4
    nc = build_nc(L)
    x = np.ascontiguousarray(inputs["x"], dtype=np.float32)
    B = x.shape[0]
    in_maps = []
    for b in range(B):
        m = {"x": x[b]}
        for n, _ in WNAMES:
            m[n] = np.ascontiguousarray(inputs[n], dtype=np.float32)
        in_maps.append(m)
    res = run_bass_kernel_spmd(nc, in_maps, core_ids=list(range(B)))
    return np.stack([res.results[b]["out"] for b in range(B)], axis=0).astype(np.float32)
```

```python
from contextlib import ExitStack
import numpy as np
import concourse.bass as bass
import concourse.mybir as mybir
from concourse.bass_utils import run_bass_kernel_spmd

F32 = mybir.dt.float32
BF16 = mybir.dt.bfloat16
ALU = mybir.AluOpType
AF = mybir.ActivationFunctionType
AX = mybir.AxisListType

S = 2048
D = 2048
NT = 16
DPROJ = 5916
BIG = 30000.0
NEG = -1.0e30

WNAMES = [
    ("pre_norm_g", [D]), ("w_in", [D, DPROJ]), ("gla_w_fg2", [16, 256]), ("gla_b_fg2", [256]),
    ("lru_conv_w", [4, 512]), ("lru_conv_b", [512]), ("lru_w_a", [4, 128, 128]), ("lru_b_a", [512]),
    ("lru_w_x", [4, 128, 128]), ("lru_b_x", [512]), ("lru_lambda", [512]),
    ("nsa_cmp_pos_k", [32, 128]), ("nsa_cmp_w1_k", [4096, 128]), ("nsa_cmp_w2_k", [128, 128]),
    ("nsa_cmp_pos_v", [32, 128]), ("nsa_cmp_w1_v", [4096, 128]), ("nsa_cmp_w2_v", [128, 128]),
    ("conf_dw_w", [31, 512]), ("conf_dw_b", [512]), ("conf_ln_g", [512]), ("conf_ln_b", [512]),
    ("conf_pw_w", [512, 512]), ("conf_pw_b", [512]), ("branch_norm_g", [D]), ("w_out", [D, D]),
    ("post_norm_g", [D]),
]


class Buf:
    __slots__ = ("name", "w", "r")

    def __init__(self, name=""):
        self.name = name
        self.w = None
        self.r = []


class Prog:
    EPOCH = 2000
    NDS = 10

    def __init__(self, nc):
        self.nc = nc
        self.engs = ["pe", "act", "dve", "pool", "sp"]
        self.ops = {e: [] for e in self.engs}
        self.cnt = {e: 0 for e in self.engs}
        self.dma_n = {"sp": 0, "pool": 0, "act": 0}
        self.waited = {e: {} for e in self.engs}
        self.last = {}

    def _deps(self, eng, reads, writes):
        deps = []
        for b in reads:
            if b.w is not None:
                deps.append(b.w)
        for b in writes:
            if b.w is not None:
                deps.append(b.w)
            for ev in b.r:
                if ev[0][0] != eng:
                    deps.append(ev)
        return deps

    def _update(self, reads, writes, ev):
        for b in reads:
            b.r.append(ev)
        for b in writes:
            b.w = ev
            b.r = []

    def _record(self, eng, deps, fn, ev, inc):
        w = self.waited[eng]
        need = {}
        for (k, v) in deps:
            if eng == "pe" and k[0] == "pe":
                continue
            if w.get(k, 0) >= v:
                continue
            if need.get(k, 0) < v:
                need[k] = v
        for k, v in need.items():
            w[k] = v
        self.ops[eng].append((list(need.items()), fn, ev[0] if ev else None, inc))
        if ev:
            self.last[ev[0]] = ev[1]

    def op(self, eng, fn, reads=(), writes=()):
        deps = self._deps(eng, reads, writes)
        c = self.cnt[eng]
        self.cnt[eng] = c + 1
        ev = ((eng, c // self.EPOCH), c % self.EPOCH + 1)
        self._record(eng, deps, fn, ev, 1)
        self._update(reads, writes, ev)

    def dma(self, q, fn, reads=(), writes=()):
        n = self.dma_n[q]
        self.dma_n[q] = n + 1
        slot = n % self.NDS
        val = 16 * (n // self.NDS + 1)
        key = ("d" + q, slot)
        deps = self._deps(q, reads, writes)
        if val > 16:
            deps.append((key, val - 16))
        ev = (key, val)
        self._record(q, deps, fn, ev, 16)
        self._update(reads, writes, ev)

    def barrier(self):
        evs = list(self.last.items())
        for e in self.engs:
            self._record(e, evs, None, None, 0)

    def emit(self):
        nc = self.nc
        keys = set()
        for e in self.engs:
            for waits, fn, key, inc in self.ops[e]:
                if key is not None:
                    keys.add(key)
                for k, v in waits:
                    keys.add(k)
        sems = {}
        with ExitStack() as st:
            for k in sorted(keys, key=str):
                sems[k] = st.enter_context(nc.semaphore("s_" + str(k[0]) + "_" + str(k[1])))
            blk = st.enter_context(nc.Block())

            def mk(en):
                def body(e):
                    for waits, fn, key, inc in self.ops[en]:
                        for (k, v) in waits:
                            e.wait_ge(sems[k], v)
                        if fn is not None:
                            fn(e).then_inc(sems[key], inc)
                return body

            blk.tensor(mk("pe"))
            blk.scalar(mk("act"))
            blk.vector(mk("dve"))
            blk.gpsimd(mk("pool"))
            blk.sync(mk("sp"))


class Builder:
    def __init__(self, nc, L, upto=99, dbg=False):
        self.nc = nc
        self.L = L
        self.upto = upto
        self.dbg = dbg
        self.P = Prog(nc)
        self.rr = 0

    def alloc(self, shape, dt=BF16):
        n = int(np.prod(shape))
        nb = n * (2 if dt == F32 else 1)
        nb = (nb + 1) // 2 * 2
        assert self.off + nb <= self.ACOLS, ("arena overflow", self.off, nb)
        a = self.arena[:, self.off:self.off + nb]
        self.off += nb
        if dt == F32:
            a = a.bitcast(F32)
        if len(shape) == 2:
            a = a.rearrange("p (a b) -> p a b", a=shape[0])
        elif len(shape) == 3:
            a = a.rearrange("p (a b c) -> p a b c", a=shape[0], b=shape[1])
        return a

    def mm(self, out, lhsT, rhs, start, stop, r, w, **kw):
        self.P.op("pe", lambda e: e.matmul(out, lhsT=lhsT, rhs=rhs, start=start, stop=stop, **kw), reads=r, writes=w)

    def tr(self, out, in_, ident, r, w):
        self.P.op("pe", lambda e: e.transpose(out=out, in_=in_, identity=ident), reads=r, writes=w)

    def act(self, out, in_, func, r, w, **kw):
        self.P.op("act", lambda e: e.activation(out=out, in_=in_, func=func, **kw), reads=r, writes=w)

    def cp(self, eng, out, in_, r, w):
        if eng == "act":
            self.act(out, in_, AF.Copy, r, w)
        else:
            self.P.op(eng, lambda e: e.tensor_copy(out=out, in_=in_), reads=r, writes=w)

    def evac(self, out, in_, r, w):
        self.rr ^= 1
        self.cp("act" if self.rr else "dve", out, in_, r, w)

    def tt(self, eng, out, in0, in1, op, r, w):
        self.P.op(eng, lambda e: e.tensor_tensor(out=out, in0=in0, in1=in1, op=op), reads=r, writes=w)

    def ts(self, eng, out, in0, s1, s2, op0, op1, r, w):
        if op1 is None:
            self.P.op(eng, lambda e: e.tensor_scalar(out=out, in0=in0, scalar1=s1, scalar2=None, op0=op0), reads=r, writes=w)
        else:
            self.P.op(eng, lambda e: e.tensor_scalar(out=out, in0=in0, scalar1=s1, scalar2=s2, op0=op0, op1=op1), reads=r, writes=w)

    def stt(self, eng, out, in0, scalar, in1, op0, op1, r, w):
        self.P.op(eng, lambda e: e.scalar_tensor_tensor(out=out, in0=in0, scalar=scalar, in1=in1, op0=op0, op1=op1), reads=r, writes=w)

    def memset(self, eng, ap, v, w):
        self.P.op(eng, lambda e: e.memset(ap, v), writes=w)

    def asel(self, out, in_, pattern, cmp, fill, base, cm, r, w):
        self.P.op("pool", lambda e: e.affine_select(out=out, in_=in_, pattern=pattern, compare_op=cmp, fill=fill, base=base, channel_multiplier=cm), reads=r, writes=w)

    def dma(self, q, out, in_, r, w):
        self.P.dma(q, lambda e: e.dma_start(out=out, in_=in_), reads=r, writes=w)

    def rsqrt(self, t, r_w):
        self.act(t, t, AF.Ln, [r_w], [r_w])
        self.act(t, t, AF.Exp, [r_w], [r_w], scale=-0.5)

    def build(self):
        nc = self.nc
        L = self.L
        self.x_d = nc.dram_tensor("x", [S, D], F32, kind="ExternalInput").ap()
        self.w = {}
        for n, shp in WNAMES:
            self.w[n] = nc.dram_tensor(n, [L] + shp, F32, kind="ExternalInput").ap()
        self.out_d = nc.dram_tensor("out", [S, D], F32, kind="ExternalOutput").ap()
        self.mixT_d = nc.dram_tensor("mixT", [D, S], BF16, kind=("ExternalOutput" if self.dbg else "Internal")).ap()
        self.B_out = [Buf("out%d" % t) for t in range(NT)]
        self.B_mixd = [Buf("mixd%d" % i) for i in range(4)]
        with ExitStack() as st:
            self.ACOLS = 106200
            self.arena = st.enter_context(nc.sbuf_tensor("arena", [128, self.ACOLS], BF16))
            self.ps = [st.enter_context(nc.psum_tensor("ps%d" % i, [128, 512], F32))[:] for i in range(8)]
            self.off = 0
            self.consts()
            self.base_off = self.off
            for l in range(L):
                self.layer(l)
            self.P.barrier()
            self.P.emit()
        return nc

    def consts(self):
        A = self.alloc
        self.identf = A([128], F32)
        self.ident = A([128], BF16)
        self.ones = A([128], BF16)
        self.triI = A([128], F32)
        self.triS = A([128], F32)
        self.mask4 = A([4, 128], F32)
        self.hT = A([16, S], BF16)
        self.wbuf = [A([16, 512], BF16), A([16, 512], BF16)]
        self.gvec = A([D], F32)
        self.bgbc = A([1024], F32)
        self.rows = A([512], F32)
        self.colv = A([4, 48], F32)
        self.B_c = Buf("consts")
        self.B_hT = [Buf("hT%d" % t) for t in range(NT)]
        self.B_w = [Buf("w0"), Buf("w1")]
        self.B_g = Buf("gvec")
        self.B_bg = Buf("bgbc")
        self.B_rows = Buf("rows")
        self.B_colv = Buf("colv")
        self.B_ps = [Buf("ps%d" % i) for i in range(8)]
        self.wslot = 0
        c = [self.B_c]
        self.memset("pool", self.identf, 0.0, c)
        self.asel(self.identf, self.identf, [[-1, 128]], ALU.not_equal, 1.0, 0, 1, c, c)
        self.cp("pool", self.ident, self.identf, c, c)
        self.memset("pool", self.ones, 1.0, c)
        self.memset("pool", self.triI, 1.0, c)
        self.asel(self.triI, self.triI, [[1, 128]], ALU.is_ge, 0.0, 0, -1, c, c)
        self.memset("pool", self.triS, 1.0, c)
        self.asel(self.triS, self.triS, [[-1, 128]], ALU.is_ge, 0.0, -1, 1, c, c)
        for h in range(4):
            self.cp("pool", self.mask4[:, h, :], self.triI, c, c)
        self.P.barrier()

    def prefetch(self, src, ncols, key):
        self.pref = (key, self.load_w(src, ncols))

    def load_w(self, src, ncols, key=None):
        if key is not None and getattr(self, "pref", None) is not None and self.pref[0] == key:
            r = self.pref[1]
            self.pref = None
            return r
        s = self.wslot
        self.wslot ^= 1
        dst = self.wbuf[s][:, :, 0:ncols]
        self.dma("pool", dst, src.rearrange("(k p) c -> p k c", p=128), [], [self.B_w[s]])
        return self.wbuf[s], self.B_w[s]

    def proj_fm(self, wap, wb, c0, m, dst_fn, pbase, func=AF.Copy, scale=1.0):
        banks = [pbase + i for i in range(4)]
        for kc in range(16):
            for tb in range(4):
                self.mm(self.ps[banks[tb]][0:m, :], wap[:, kc, c0:c0 + m], self.hT[:, kc, tb * 512:(tb + 1) * 512],
                        kc == 0, kc == 15, [wb] + self.B_hT[tb * 4:tb * 4 + 4], [self.B_ps[banks[tb]]])
        for tb in range(4):
            out, ob = dst_fn(tb)
            if func == AF.Copy and scale == 1.0:
                self.evac(out, self.ps[banks[tb]][0:m, :], [self.B_ps[banks[tb]]], [ob])
            else:
                self.act(out, self.ps[banks[tb]][0:m, :], func, [self.B_ps[banks[tb]]], [ob], scale=scale)

    def proj_tm(self, wap, wb, c0, n, dst_fn, func=AF.Copy):
        for tt in range(NT):
            b = tt % 4
            for kc in range(16):
                self.mm(self.ps[b][:, 0:n], self.hT[:, kc, tt * 128:(tt + 1) * 128], wap[:, kc, c0:c0 + n],
                        kc == 0, kc == 15, [wb, self.B_hT[tt]], [self.B_ps[b]])
            out, ob = dst_fn(tt)
            if func == AF.Copy:
                self.evac(out, self.ps[b][:, 0:n], [self.B_ps[b]], [ob])
            else:
                self.act(out, self.ps[b][:, 0:n], func, [self.B_ps[b]], [ob])

    def layer(self, l):
        P = self.P
        W = {n: self.w[n][l] for n, _ in WNAMES}
        self.W = W
        src_d = self.x_d if l == 0 else self.out_d
        self.off = self.base_off
        A = self.alloc
        self.dma("sp", self.gvec, W["pre_norm_g"].partition_broadcast(128), [], [self.B_g])
        self.dma("sp", self.bgbc[:, 0:512], W["branch_norm_g"][0:512].partition_broadcast(128), [], [self.B_bg])
        self.dma("sp", self.bgbc[:, 512:1024], W["branch_norm_g"][1024:1536].partition_broadcast(128), [], [self.B_bg])
        rowsrc = [("lru_conv_w", 4), ("lru_conv_b", 1), ("lru_b_a", 1), ("lru_b_x", 1), ("lru_lambda", 1),
                  ("conf_dw_w", 31), ("conf_dw_b", 1), ("conf_ln_g", 1), ("conf_ln_b", 1), ("conf_pw_b", 1)]
        self.ROW = {}
        r0 = 0
        for n, k in rowsrc:
            src = W[n] if k > 1 else W[n].rearrange("(o c) -> o c", o=1)
            self.dma("sp", self.rows[r0:r0 + k, :], src, [], [self.B_rows])
            self.ROW[n] = r0
            r0 += k
        self.dma("sp", self.rows[r0:r0 + 1, :], W["branch_norm_g"][512:1024].rearrange("(o c) -> o c", o=1), [], [self.B_rows])
        self.ROW["bg_lru"] = r0
        r0 += 1
        self.dma("sp", self.rows[r0:r0 + 1, :], W["branch_norm_g"][1536:2048].rearrange("(o c) -> o c", o=1), [], [self.B_rows])
        self.ROW["bg_conf"] = r0
        r0 += 1
        NR = r0
        for c in range(4):
            self.tr(self.ps[7][:, c * 48:c * 48 + NR], self.rows[0:NR, c * 128:(c + 1) * 128], self.identf[0:NR, 0:NR],
                    [self.B_rows, self.B_c], [self.B_ps[7]])
        for c in range(4):
            self.cp("dve", self.colv[:, c, 0:NR], self.ps[7][:, c * 48:c * 48 + NR], [self.B_ps[7]], [self.B_colv])

        m0 = self.off
        if l == 0:
            self.prefetch(W["w_in"][:, 0:512], 512, (0, 0))
        fused_in = (l > 0) and self.upto >= 5
        xt = [A([D], F32), A([D], F32)]
        hn = [A([D], BF16), A([D], BF16)]
        junk = A([D], BF16)
        st1 = A([NT, 2], F32)
        B_xt = [Buf(), Buf()]
        B_hn = [Buf(), Buf()]
        B_junk = Buf()
        B_st = [Buf() for _ in range(NT)]
        for t in range(NT if not fused_in else 0):
            i = t % 2
            self.dma("sp", xt[i], src_d[t * 128:(t + 1) * 128, :], [self.B_out[t]], [B_xt[i]])
            ss = st1[:, t, 0:1]
            self.act(junk, xt[i], AF.Square, [B_xt[i]], [B_junk, B_st[t]], accum_out=ss)
            self.ts("dve", ss, ss, 1.0 / D, 1e-6, ALU.mult, ALU.add, [B_st[t]], [B_st[t]])
            self.rsqrt(ss, B_st[t])
            self.stt("dve", hn[i], xt[i], ss, self.gvec, ALU.mult, ALU.mult, [B_xt[i], B_st[t], self.B_g], [B_hn[i]])
            pA = self.ps[2 * i].bitcast(BF16)
            pB = self.ps[2 * i + 1].bitcast(BF16)
            for kc in range(16):
                dst = (pA if kc < 8 else pB)[:, (kc % 8) * 128:(kc % 8 + 1) * 128]
                self.tr(dst, hn[i][:, kc * 128:(kc + 1) * 128], self.ident, [B_hn[i], self.B_c],
                        [self.B_ps[2 * i + (0 if kc < 8 else 1)]])
            self.cp("act", self.hT[:, 0:8, t * 128:(t + 1) * 128], pA.rearrange("p (a b) -> p a b", a=8),
                    [self.B_ps[2 * i]], [self.B_hT[t]])
            self.cp("dve", self.hT[:, 8:16, t * 128:(t + 1) * 128], pB.rearrange("p (a b) -> p a b", a=8),
                    [self.B_ps[2 * i + 1]], [self.B_hT[t]])
        P.barrier()
        self.off = m0
        if self.upto >= 1:
            self.gla(l)
            P.barrier()
            self.off = m0
        if self.upto >= 2:
            self.lru(l)
            P.barrier()
            self.off = m0
        if self.upto >= 3:
            self.nsa(l)
            P.barrier()
            self.off = m0
        if self.upto >= 4:
            self.conf(l)
            P.barrier()
            self.off = m0
        if self.upto >= 5:
            self.outproj(l, src_d)
            P.barrier()
            self.off = m0

    def tm_tail_alloc(self):
        A = self.alloc
        self.tl = dict(sq=A([512], F32), st=A([2, 4], F32), gz=A([2, 512], F32), mx=A([2, 512], BF16), ms=A([2, 512], BF16))
        self.B_tl = dict(sq=Buf(), st=[Buf(), Buf()], gz=[Buf(), Buf()], mx=[Buf(), Buf()], ms=[Buf(), Buf()])

    def tm_tail(self, o_sb, B_o, z_ap, B_z, gcol0, mixer, t, pbank):
        self.tm_tail_a(o_sb, B_o, z_ap, B_z, gcol0, t)
        self.tm_tail_b(mixer, t, pbank)

    def tm_tail_a(self, o_sb, B_o, z_ap, B_z, gcol0, t):
        self.tm_tail_a1(o_sb, B_o, t)
        self.tm_tail_a2(t)
        self.tm_tail_a3(o_sb, B_o, z_ap, B_z, gcol0, t)

    def tm_tail_a1(self, o_sb, B_o, t):
        i = t % 2
        tl, Bt = self.tl, self.B_tl
        self.act(tl["sq"], o_sb, AF.Square, [B_o], [Bt["sq"]])
        st = tl["st"][:, i, :]
        self.P.op("dve", lambda e: e.tensor_reduce(out=st, in_=tl["sq"].rearrange("p (h e) -> p h e", h=4), axis=AX.X, op=ALU.add),
                  reads=[Bt["sq"]], writes=[Bt["st"][i]])
        self.ts("dve", st, st, 1.0 / 128, 1e-6, ALU.mult, ALU.add, [Bt["st"][i]], [Bt["st"][i]])

    def tm_tail_a2(self, t):
        i = t % 2
        self.rsqrt(self.tl["st"][:, i, :], self.B_tl["st"][i])

    def tm_tail_a3(self, o_sb, B_o, z_ap, B_z, gcol0, t):
        i = t % 2
        tl, Bt = self.tl, self.B_tl
        st = tl["st"][:, i, :]
        gz = tl["gz"][:, i, :]
        self.tt("pool", gz, z_ap, self.bgbc[:, gcol0:gcol0 + 512], ALU.mult, [B_z, self.B_bg], [Bt["gz"][i]])
        mx = tl["mx"][:, i, :]
        self.tt("dve", gz.rearrange("p (h e) -> p h e", h=4), gz.rearrange("p (h e) -> p h e", h=4),
                st.unsqueeze(2).to_broadcast([128, 4, 128]), ALU.mult, [Bt["st"][i], Bt["gz"][i]], [Bt["gz"][i]])
        self.tt("dve", mx, o_sb, gz, ALU.mult, [B_o, Bt["gz"][i]], [Bt["mx"][i]])

    def tm_tail_b(self, mixer, t, pbank):
        i = t % 2
        tl, Bt = self.tl, self.B_tl
        mx = tl["mx"][:, i, :]
        pT = self.ps[pbank].bitcast(BF16)
        for h in range(4):
            self.tr(pT[:, h * 128:(h + 1) * 128], mx[:, h * 128:(h + 1) * 128], self.ident, [Bt["mx"][i], self.B_c], [self.B_ps[pbank]])
        ms = tl["ms"][:, i, :]
        self.evac(ms, pT[:, 0:512], [self.B_ps[pbank]], [Bt["ms"][i]])
        dst = self.mixT_d[mixer * 512:(mixer + 1) * 512, t * 128:(t + 1) * 128].rearrange("(c p) j -> p c j", p=128)
        self.dma("sp", dst, ms.rearrange("p (c j) -> p c j", c=4), [Bt["ms"][i]], [self.B_mixd[mixer]])

    def gla(self, l):
        A = self.alloc
        W = self.W
        win = W["w_in"]
        qT = A([2, S], BF16)
        kT = A([2, S], BF16)
        ktm = A([NT, 256], BF16)
        vtm = A([NT, 512], BF16)
        ztm = A([NT, 512], BF16)
        fgT = A([S], BF16)
        w2a = A([256], BF16)
        B_q, B_k, B_ktm, B_v, B_z, B_fg, B_w2 = Buf(), Buf(), [Buf() for _ in range(NT)], [Buf() for _ in range(NT)], [Buf() for _ in range(NT)], Buf(), Buf()
        self.memset("pool", fgT[0:32, :], 1.0, [B_fg])
        self.dma("pool", w2a[0:16, :], W["gla_w_fg2"], [], [B_w2])
        self.dma("pool", w2a[16:17, :], W["gla_b_fg2"].rearrange("(o c) -> o c", o=1), [], [B_w2])
        wap, wb = self.load_w(win[:, 0:512], 512, key=(l, 0))
        for g in range(2):
            self.proj_fm(wap, wb, g * 128, 128, lambda tb, g=g: (qT[:, g, tb * 512:(tb + 1) * 512], B_q), 0 if g == 0 else 4, func=AF.Copy, scale=0.125)
        for g in range(2):
            self.proj_fm(wap, wb, 256 + g * 128, 128, lambda tb, g=g: (kT[:, g, tb * 512:(tb + 1) * 512], B_k), 0 if g == 0 else 4)
        for t in range(NT):
            bk = t % 2
            pT = self.ps[bk].bitcast(BF16)
            for dc in range(2):
                self.tr(pT[:, dc * 128:(dc + 1) * 128], kT[:, dc, t * 128:(t + 1) * 128], self.ident, [B_k, self.B_c], [self.B_ps[bk]])
            self.evac(ktm[:, t, :], pT[:, 0:256], [self.B_ps[bk]], [B_ktm[t]])
        wap, wb = self.load_w(win[:, 512:1024], 512)
        self.proj_tm(wap, wb, 0, 512, lambda t: (vtm[:, t, :], B_v[t]))
        wap, wb = self.load_w(win[:, 1024:1040], 16)
        self.proj_fm(wap, wb, 0, 16, lambda tb: (fgT[0:16, tb * 512:(tb + 1) * 512], B_fg), 4)
        wap, wb = self.load_w(win[:, 1040:1552], 512)
        self.proj_tm(wap, wb, 0, 512, lambda t: (ztm[:, t, :], B_z[t]), func=AF.Silu)
        self.prefetch(win[:, 1552:2064], 512, (l, 1552))
        Lf = A([2, 256], F32)
        eq = A([2, 256], F32)
        ek = A([2, 256], F32)
        ekk = A([2, 256], F32)
        qd = A([2, 4, 128], BF16)
        kd = A([2, 256], BF16)
        kk = A([2, 256], BF16)
        at = A([2, 512], BF16)
        Sf = A([2, 128], F32)
        Sb = A([2, 128], BF16)
        osb = A([2, 512], F32)
        dec = A([2, 2], F32)
        B_L, B_eq, B_ek, B_ekk, B_qd, B_kd, B_kk, B_at, B_os, B_dec = ([Buf(), Buf()] for _ in range(10))
        B_Sf, B_Sb = Buf(), Buf()
        self.tm_tail_alloc()
        self.memset("pool", Sf, 0.0, [B_Sf])
        self.memset("pool", Sb, 0.0, [B_Sb])
        self.memset("pool", qd, 0.0, B_qd)
        ps, Bp = self.ps, self.B_ps
        def front(t):
                i = t % 2
                tsl = slice(t * 128, (t + 1) * 128)
                self.mm(ps[0][:, 0:256], fgT[0:17, tsl], w2a[0:17, :], True, True, [B_fg, B_w2], [Bp[0]])
                self.act(Lf[:, i, :], ps[0][:, 0:256], AF.Exp, [Bp[0]], [B_L[i]], scale=-1.0)
                self.act(Lf[:, i, :], Lf[:, i, :], AF.Ln, [B_L[i]], [B_L[i]], bias=1.0)
                for dc in range(2):
                    self.mm(ps[1][:, dc * 128:(dc + 1) * 128], Lf[:, i, dc * 128:(dc + 1) * 128], self.triI, True, True, [B_L[i], self.B_c], [Bp[1]])
                self.act(eq[:, i, :], ps[1][:, 0:256], AF.Exp, [Bp[1]], [B_eq[i]], scale=-1.0 / 16)
                self.act(ek[:, i, :], ps[1][:, 0:256], AF.Exp, [Bp[1]], [B_ek[i]], scale=1.0 / 16)
                self.mm(ps[2][:, 0:256], self.triS, Lf[:, i, :], True, True, [B_L[i], self.B_c], [Bp[2]])
                self.act(ekk[:, i, :], ps[2][:, 0:256], AF.Exp, [Bp[2]], [B_ekk[i]], scale=-1.0 / 16)
                for h in range(4):
                    dc, pb = h // 2, (h % 2) * 64
                    self.tt("dve", qd[pb:pb + 64, i, h, :], qT[pb:pb + 64, dc, tsl], eq[pb:pb + 64, i, dc * 128:(dc + 1) * 128], ALU.mult, [B_q, B_eq[i]], [B_qd[i]])
                for dc in range(2):
                    self.tt("pool", kd[:, i, dc * 128:(dc + 1) * 128], kT[:, dc, tsl], ek[:, i, dc * 128:(dc + 1) * 128], ALU.mult, [B_k, B_ek[i]], [B_kd[i]])
                self.tt("pool", kk[:, i, :], ktm[:, t, :], ekk[:, i, :], ALU.mult, [B_ktm[t], B_ekk[i]], [B_kk[i]])
                self.cp("dve", dec[:, i, :], eq[:, i, :].rearrange("p (a b) -> p a b", a=2)[:, :, 127], [B_eq[i]], [B_dec[i]])

        def back(t):
                i = t % 2
                tsl = slice(t * 128, (t + 1) * 128)
                for h in range(4):
                    dc, pb = h // 2, (h % 2) * 64
                    self.mm(ps[3][:, h * 128:(h + 1) * 128], kd[:, i, dc * 128:(dc + 1) * 128], qd[:, i, h, :],
                            h == 0, h == 3, [B_kd[i], B_qd[i]], [Bp[3]], skip_group_check=True)
                self.tt("dve", at[:, i, :], ps[3], self.mask4.rearrange("p a b -> p (a b)"), ALU.mult, [Bp[3], self.B_c], [B_at[i]])
                ob = 4 + i
                for h in range(4):
                    dc, pb = h // 2, (h % 2) * 64
                    self.mm(ps[ob][:, h * 128:(h + 1) * 128], at[:, i, h * 128:(h + 1) * 128], vtm[:, t, h * 128:(h + 1) * 128],
                            h == 0, False, [B_at[i], B_v[t]], [Bp[ob]], skip_group_check=True)
                    self.mm(ps[ob][:, h * 128:(h + 1) * 128], qd[:, i, h, :], Sb[:, dc, :],
                            False, h == 3, [B_qd[i], B_Sb], [Bp[ob]], skip_group_check=True)
                if t >= 2:
                    self.tm_tail_b(0, t - 2, 7)
                self.cp("act", osb[:, i, :], ps[ob], [Bp[ob]], [B_os[i]])
                if t < NT - 1:
                    for h in range(4):
                        dc = h // 2
                        self.mm(ps[6][:, h * 128:(h + 1) * 128], kk[:, i, dc * 128:(dc + 1) * 128], vtm[:, t, h * 128:(h + 1) * 128],
                                h == 0, h == 3, [B_kk[i], B_v[t]], [Bp[6]], skip_group_check=True)
                    for h in range(4):
                        dc, pb = h // 2, (h % 2) * 64
                        self.stt("dve", Sf[pb:pb + 64, dc, :], Sf[pb:pb + 64, dc, :], dec[pb:pb + 64, i, dc:dc + 1], ps[6][pb:pb + 64, h * 128:(h + 1) * 128], ALU.mult, ALU.add,
                                 [B_Sf, B_dec[i], Bp[6]], [B_Sf])
                    self.cp("dve", Sb, Sf, [B_Sf], [B_Sb])

        for t in range(NT + 3):
            if t < NT:
                front(t)
            if 3 <= t:
                u = t - 3
                self.tm_tail_a2(u)
                self.tm_tail_a3(osb[:, u % 2, :], B_os[u % 2], ztm[:, u, :], B_z[u], 0, u)
            if 1 <= t <= NT:
                back(t - 1)
            if 2 <= t <= NT + 1:
                u = t - 2
                self.tm_tail_a1(osb[:, u % 2, :], B_os[u % 2], u)
        self.tm_tail_b(0, NT - 2, 7)
        self.tm_tail_b(0, NT - 1, 7)

    def fm_tail_alloc(self):
        A = self.alloc
        self.ft = dict(hsq=A([2, 512], BF16), rs=A([2, 512], F32), mo=A([2, 512], BF16))
        self.B_ft = dict(hsq=[Buf(), Buf()], rs=[Buf(), Buf()], mo=[Buf(), Buf()])

    def fm_tail(self, h_ap, B_h, z_ap, B_z, gcol, row0, tb, i, pbank):
        self.fm_tail_pre(h_ap, B_h, i)
        self.fm_tail_post(h_ap, B_h, z_ap, B_z, gcol, row0, tb, i, pbank)

    def fm_tail_pre(self, h_ap, B_h, i):
        ft, Bf = self.ft, self.B_ft
        self.act(ft["hsq"][:, i, :], h_ap, AF.Square, [B_h], [Bf["hsq"][i]])

    def fm_tail_post(self, h_ap, B_h, z_ap, B_z, gcol, row0, tb, i, pbank):
        ft, Bf = self.ft, self.B_ft
        self.mm(self.ps[pbank], self.ones, ft["hsq"][:, i, :], True, True, [Bf["hsq"][i], self.B_c], [self.B_ps[pbank]])
        rs = ft["rs"][:, i, :]
        self.ts("dve", rs, self.ps[pbank], 1.0 / 128, 1e-6, ALU.mult, ALU.add, [self.B_ps[pbank]], [Bf["rs"][i]])
        self.rsqrt(rs, Bf["rs"][i])
        self.tt("dve", rs, h_ap, rs, ALU.mult, [B_h, Bf["rs"][i]], [Bf["rs"][i]])
        self.stt("dve", ft["mo"][:, i, :], rs, gcol, z_ap, ALU.mult, ALU.mult, [Bf["rs"][i], self.B_colv, B_z], [Bf["mo"][i]])
        self.dma("sp", self.mixT_d[row0:row0 + 128, tb * 512:(tb + 1) * 512], ft["mo"][:, i, :], [Bf["mo"][i]], [self.B_mixd[row0 // 512]])

    def lru(self, l):
        A = self.alloc
        W = self.W
        win = W["w_in"]
        ps, Bp = self.ps, self.B_ps
        R = self.ROW
        cv = self.colv
        xp = A([4, S + 4], BF16)
        zT = A([4, S], BF16)
        wa = A([4, 128], BF16)
        wx = A([4, 128], BF16)
        c8 = A([4, 2], F32)
        B_xp, B_zT, B_wa, B_c8 = Buf(), Buf(), Buf(), Buf()
        self.memset("pool", xp[:, :, 0:4], 0.0, [B_xp])
        self.dma("pool", wa, W["lru_w_a"].rearrange("h i j -> i h j"), [], [B_wa])
        self.dma("pool", wx, W["lru_w_x"].rearrange("h i j -> i h j"), [], [B_wa])
        wap, wb = self.load_w(win[:, 1552:2064], 512, key=(l, 1552))
        for g in range(4):
            self.proj_fm(wap, wb, g * 128, 128, lambda tb, g=g: (xp[:, g, 4 + tb * 512:4 + (tb + 1) * 512], B_xp), 0 if g % 2 == 0 else 4)
        wap, wb = self.load_w(win[:, 2064:2576], 512)
        for g in range(4):
            self.proj_fm(wap, wb, g * 128, 128, lambda tb, g=g: (zT[:, g, tb * 512:(tb + 1) * 512], B_zT), 0 if g % 2 == 0 else 4, func=AF.Silu)
        lam = cv[:, :, R["lru_lambda"]]
        self.act(c8[:, :, 0], lam, AF.Exp, [self.B_colv], [B_c8], scale=-1.0)
        self.act(c8[:, :, 0], c8[:, :, 0], AF.Ln, [B_c8], [B_c8], bias=1.0)
        self.ts("dve", c8[:, :, 1], c8[:, :, 0], -16.0, None, ALU.mult, None, [B_c8], [B_c8])
        self.ts("dve", c8[:, :, 0], c8[:, :, 0], -8.0, None, ALU.mult, None, [B_c8], [B_c8])
        self.prefetch(win[:, 2576:3088], 512, (l, 2576))
        xcs = [A([S], F32), A([S], F32)]
        rA = A([S], F32)
        ig = A([S], F32)
        a2 = A([S], F32)
        hb = A([S], F32)
        xcb = A([S], BF16)
        mo = A([S], BF16)
        B_xcs = [Buf(), Buf()]
        B_r, B_ig, B_a2, B_h, B_xcb, B_mo = (Buf() for _ in range(6))
        cw = R["lru_conv_w"]

        def conv_main(c):
            xc, Bx = xcs[c % 2], B_xcs[c % 2]
            self.act(xc, xp[:, c, 1:1 + S], AF.Identity, [B_xp, self.B_colv], [Bx],
                     scale=cv[:, c, cw:cw + 1], bias=cv[:, c, R["lru_conv_b"]:R["lru_conv_b"] + 1])
            for k in range(1, 4):
                self.stt("dve", xc, xp[:, c, 1 + k:1 + k + S], cv[:, c, cw + k:cw + k + 1], xc, ALU.mult, ALU.add,
                         [B_xp, self.B_colv, Bx], [Bx])

        def conv_cast(c):
            self.cp("act", xcb, xcs[c % 2], [B_xcs[c % 2]], [B_xcb])

        conv_main(0)
        conv_cast(0)
        for c in range(4):
            xc, Bx = xcs[c % 2], B_xcs[c % 2]
            for tb in range(4):
                self.mm(ps[tb], wa[:, c, :], xcb[:, tb * 512:(tb + 1) * 512], True, True, [B_wa, B_xcb], [Bp[tb]])
            for tb in range(4):
                self.mm(ps[4 + tb], wx[:, c, :], xcb[:, tb * 512:(tb + 1) * 512], True, True, [B_wa, B_xcb], [Bp[4 + tb]])
            for tb in range(4):
                self.act(rA[:, tb * 512:(tb + 1) * 512], ps[tb], AF.Sigmoid, [Bp[tb], self.B_colv], [B_r], bias=cv[:, c, R["lru_b_a"]:R["lru_b_a"] + 1])
            for tb in range(4):
                self.act(ig[:, tb * 512:(tb + 1) * 512], ps[4 + tb], AF.Sigmoid, [Bp[4 + tb], self.B_colv], [B_ig], bias=cv[:, c, R["lru_b_x"]:R["lru_b_x"] + 1])
            self.act(a2, rA, AF.Exp, [B_r, B_c8], [B_a2], scale=c8[:, c, 1:2])
            self.act(rA, rA, AF.Exp, [B_r, B_c8], [B_r], scale=c8[:, c, 0:1])
            self.act(a2, a2, AF.Sqrt, [B_a2], [B_a2], scale=-1.0, bias=1.0)
            self.tt("dve", ig, ig, xc, ALU.mult, [B_ig, Bx], [B_ig])
            self.tt("dve", ig, ig, a2, ALU.mult, [B_ig, B_a2], [B_ig])
            self.P.op("dve", lambda e: e.tensor_tensor_scan(out=hb, data0=rA, data1=ig, initial=0.0, op0=ALU.mult, op1=ALU.add),
                      reads=[B_r, B_ig], writes=[B_h])
            if c + 1 < 4:
                conv_main(c + 1)
            self.act(mo, hb, AF.Square, [B_h], [B_mo])
            for tb in range(4):
                self.mm(ps[tb], self.ones, mo[:, tb * 512:(tb + 1) * 512], True, True, [B_mo, self.B_c], [Bp[tb]])
            for tb in range(4):
                self.act(a2[:, tb * 512:(tb + 1) * 512], ps[tb], AF.Ln, [Bp[tb]], [B_a2], scale=1.0 / 128, bias=1e-6)
            self.act(a2, a2, AF.Exp, [B_a2], [B_a2], scale=-0.5)
            self.tt("dve", a2, hb, a2, ALU.mult, [B_h, B_a2], [B_a2])
            self.stt("dve", mo, a2, cv[:, c, R["bg_lru"]:R["bg_lru"] + 1], zT[:, c, :], ALU.mult, ALU.mult, [B_a2, self.B_colv, B_zT], [B_mo])
            self.dma("sp", self.mixT_d[512 + c * 128:512 + (c + 1) * 128, :], mo, [B_mo], [self.B_mixd[1]])
            if c + 1 < 4:
                conv_cast(c + 1)

    def conf(self, l):
        A = self.alloc
        W = self.W
        win = W["w_in"]
        ps, Bp = self.ps, self.B_ps
        R = self.ROW
        cv = self.colv
        PADC = 32
        yp = A([4, S + PADC], BF16)
        zT = A([4, S], BF16)
        pw = A([4, 512], BF16)
        sg_off = self.off
        sg = A([4, S], BF16)
        B_yp, B_sg, B_zT, B_pw, B_dg = Buf(), Buf(), Buf(), Buf(), Buf()
        self.memset("pool", yp[:, :, 0:PADC], 0.0, [B_yp])
        self.dma("pool", pw, W["conf_pw_w"].rearrange("(k p) c -> p k c", p=128), [], [B_pw])
        wap, wb = self.load_w(win[:, 4380:4892], 512, key=(l, 4380))
        for g in range(4):
            self.proj_fm(wap, wb, g * 128, 128, lambda tb, g=g: (yp[:, g, PADC + tb * 512:PADC + (tb + 1) * 512], B_yp), 0 if g % 2 == 0 else 4)
        wap, wb = self.load_w(win[:, 4892:5404], 512)
        for g in range(4):
            self.proj_fm(wap, wb, g * 128, 128, lambda tb, g=g: (sg[:, g, tb * 512:(tb + 1) * 512], B_sg), 0 if g % 2 == 0 else 4, func=AF.Sigmoid)
        wap, wb = self.load_w(win[:, 5404:5916], 512)
        for g in range(4):
            self.proj_fm(wap, wb, g * 128, 128, lambda tb, g=g: (zT[:, g, tb * 512:(tb + 1) * 512], B_zT), 0 if g % 2 == 0 else 4, func=AF.Silu)
        for g in range(4):
            self.tt("pool" if g % 2 else "dve", yp[:, g, PADC:PADC + S], yp[:, g, PADC:PADC + S], sg[:, g, :], ALU.mult, [B_yp, B_sg], [B_yp])
        self.P.barrier()
        dgs = [self.wbuf[0][:, :, :].rearrange("p a b -> p (a b)"), self.wbuf[1][:, :, :].rearrange("p a b -> p (a b)")]

        def dg(c, k):
            idx = c * 31 + k
            return dgs[idx // 62][:, (idx % 62) * 128:(idx % 62 + 1) * 128]
        for c in range(4):
            for k in range(31):
                sc_ = cv[:, c, R["conf_dw_w"] + k:R["conf_dw_w"] + k + 1]
                if (c * 31 + k) % 2 == 0:
                    self.ts("dve", dg(c, k), self.identf, sc_, None, ALU.mult, None,
                            [self.B_c, self.B_colv, self.B_w[0], self.B_w[1]], [B_dg])
                else:
                    self.act(dg(c, k), self.identf, AF.Copy, [self.B_c, self.B_colv, self.B_w[0], self.B_w[1]], [B_dg], scale=sc_)
        if self.upto >= 5:
            mview = self.mixT_d.rearrange("(k p) t -> p k t", p=128)
            self.dma("sp", self.hT[:, 0:12, :], mview[:, 0:12, :], self.B_mixd[0:3] + self.B_hT, self.B_hT)
            self.mT_pre = True
        self.off = sg_off
        cvf = A([2, 4, 512], F32)
        cvb = A([2, 512], BF16)
        sqb = A([2, 512], BF16)
        mean = A([2, 512], F32)
        rstd = A([2, 512], F32)
        tmp = A([2, 512], F32)
        sT = A([4, 512], BF16)
        od = A([2, 512], F32)
        B_cvf = [[Buf() for _ in range(4)] for _ in range(2)]
        B_cvb, B_sqb, B_tmp, B_od, B_mean, B_rstd = ([Buf(), Buf()] for _ in range(6))
        B_sT = [Buf() for _ in range(4)]
        self.fm_tail_alloc()
        self.cn = 0

        def convstage(tb, cs=(0, 1, 2, 3)):
            j = tb % 2
            sb0 = 2 + 2 * j
            for c in cs:
                i = self.cn % 2
                self.cn += 1
                for k in range(31):
                    o0 = PADC - 30 + k + tb * 512
                    self.mm(ps[i], dg(c, k), yp[:, c, o0:o0 + 512], k == 0, k == 30, [B_dg, B_yp], [Bp[i]])
                self.act(cvf[:, j, c, :], ps[i], AF.Identity, [Bp[i], self.B_colv], [B_cvf[j][c]], bias=cv[:, c, R["conf_dw_b"]:R["conf_dw_b"] + 1])
                self.act(cvb[:, i, :], cvf[:, j, c, :], AF.Copy, [B_cvf[j][c]], [B_cvb[i]])
                self.act(sqb[:, i, :], cvf[:, j, c, :], AF.Square, [B_cvf[j][c]], [B_sqb[i]])
                self.mm(ps[sb0], self.ones, cvb[:, i, :], c == 0, c == 3, [self.B_c, B_cvb[i]], [Bp[sb0]])
                self.mm(ps[sb0 + 1], self.ones, sqb[:, i, :], c == 0, c == 3, [self.B_c, B_sqb[i]], [Bp[sb0 + 1]])

        def lnstage(tb):
            j = tb % 2
            sb0 = 2 + 2 * j
            mn, rs_ = mean[:, j, :], rstd[:, j, :]
            self.ts("dve", mn, ps[sb0], 1.0 / 512, None, ALU.mult, None, [Bp[sb0]], [B_mean[j]])
            self.ts("dve", rs_, ps[sb0 + 1], 1.0 / 512, 1e-6, ALU.mult, ALU.add, [Bp[sb0 + 1]], [B_rstd[j]])
            self.tt("dve", tmp[:, 0, :], mn, mn, ALU.mult, [B_mean[j]], [B_tmp[0]])
            self.tt("dve", rs_, rs_, tmp[:, 0, :], ALU.subtract, [B_rstd[j], B_tmp[0]], [B_rstd[j]])
            self.rsqrt(rs_, B_rstd[j])
            for c in range(4):
                i = c % 2
                self.tt("dve", tmp[:, i, :], cvf[:, j, c, :], mn, ALU.subtract, [B_cvf[j][c], B_mean[j]], [B_tmp[i]])
                self.tt("dve", tmp[:, i, :], tmp[:, i, :], rs_, ALU.mult, [B_tmp[i], B_rstd[j]], [B_tmp[i]])
                self.act(sT[:, c, :], tmp[:, i, :], AF.Silu, [B_tmp[i], self.B_colv], [B_sT[c]],
                         scale=cv[:, c, R["conf_ln_g"]:R["conf_ln_g"] + 1], bias=cv[:, c, R["conf_ln_b"]:R["conf_ln_b"] + 1])

        def pwstage(tb, jc):
            i = jc % 2
            for ic in range(4):
                self.mm(ps[6], pw[:, ic, jc * 128:(jc + 1) * 128], sT[:, ic, :], ic == 0, ic == 3, [B_pw, B_sT[ic]], [Bp[6]])
            self.act(od[:, i, :], ps[6], AF.Identity, [Bp[6], self.B_colv], [B_od[i]], bias=cv[:, jc, R["conf_pw_b"]:R["conf_pw_b"] + 1])
            self.fm_tail_pre(od[:, i, :], B_od[i], i)

        def tlstage(tb, jc):
            i = jc % 2
            self.fm_tail_post(od[:, i, :], B_od[i], zT[:, jc, tb * 512:(tb + 1) * 512], B_zT, cv[:, jc, R["bg_conf"]:R["bg_conf"] + 1], 1536 + jc * 128, tb, i, 7)

        convstage(0)
        for tb in range(4):
            nx = tb + 1 < 4
            lnstage(tb)
            if nx:
                convstage(tb + 1, (0, 1))
            pwstage(tb, 0)
            if nx:
                convstage(tb + 1, (2,))
            tlstage(tb, 0)
            pwstage(tb, 1)
            if nx:
                convstage(tb + 1, (3,))
            tlstage(tb, 1)
            pwstage(tb, 2)
            tlstage(tb, 2)
            pwstage(tb, 3)
            tlstage(tb, 3)

    def nsa(self, l):
        A = self.alloc
        W = self.W
        win = W["w_in"]
        ps, Bp = self.ps, self.B_ps
        qT = A([4, S], BF16)
        kcT = A([S], BF16)
        vcT = A([S], BF16)
        ksT = A([S], BF16)
        kwT = A([S], BF16)
        vs = A([NT, 130], BF16)
        vw = A([NT, 130], BF16)
        ztm = A([NT, 512], BF16)
        gt = A([NT, 12], F32)
        EX = A([S], BF16)
        kcmpT = A([128], BF16)
        vaug = A([162], BF16)
        mlp_off = self.off
        posr = A([128], F32)
        posT = A([64], BF16)
        w2k = A([128], BF16)
        w2v = A([128], BF16)
        hid = A([2, 128], BF16)
        cb = A([2], F32)
        B_q, B_kc, B_vc, B_ks, B_kw, B_EX, B_pos, B_w2, B_kcmp, B_vaug, B_hid, B_cb = (Buf() for _ in range(12))
        B_vs = [Buf() for _ in range(NT)]
        B_vw = [Buf() for _ in range(NT)]
        B_z = [Buf() for _ in range(NT)]
        B_gt = [Buf() for _ in range(NT)]
        self.memset("pool", vs, 1.0, B_vs)
        self.memset("pool", vw, 1.0, B_vw)
        self.memset("pool", EX, BIG, [B_EX])
        self.asel(EX, EX, [[1, S]], ALU.is_ge, 0.0, 0, -64, [B_EX], [B_EX])
        self.asel(EX, EX, [[-1, S]], ALU.is_ge, 0.0, 63, 64, [B_EX], [B_EX])
        self.memset("pool", vaug, 1.0, [B_vaug])
        self.asel(vaug[:, 130:162], vaug[:, 130:162], [[-4, 32]], ALU.is_ge, 0.0, 1, 1, [B_vaug], [B_vaug])
        self.asel(vaug[:, 130:162], vaug[:, 130:162], [[4, 32]], ALU.is_ge, 0.0, 3, -1, [B_vaug], [B_vaug])
        self.dma("sp", posr[0:32, :], W["nsa_cmp_pos_k"], [], [B_pos])
        self.dma("sp", posr[32:64, :], W["nsa_cmp_pos_v"], [], [B_pos])
        self.dma("pool", w2k, W["nsa_cmp_w2_k"], [], [B_w2])
        self.dma("pool", w2v, W["nsa_cmp_w2_v"], [], [B_w2])
        wap, wb = self.load_w(win[:, 2576:3088], 512, key=(l, 2576))
        for g in range(4):
            self.proj_fm(wap, wb, g * 128, 128, lambda tb, g=g: (qT[:, g, tb * 512:(tb + 1) * 512], B_q), 0 if g % 2 == 0 else 4, func=AF.Copy, scale=128.0 ** -0.5)
        wap, wb = self.load_w(win[:, 3088:3600], 512)
        self.proj_fm(wap, wb, 0, 128, lambda tb: (kcT[:, tb * 512:(tb + 1) * 512], B_kc), 0)
        self.proj_fm(wap, wb, 128, 128, lambda tb: (vcT[:, tb * 512:(tb + 1) * 512], B_vc), 4)
        self.proj_fm(wap, wb, 256, 128, lambda tb: (ksT[:, tb * 512:(tb + 1) * 512], B_ks), 0)
        self.proj_tm(wap, wb, 384, 128, lambda t: (vs[:, t, 0:128], B_vs[t]))
        wap, wb = self.load_w(win[:, 3600:3868], 268)
        self.proj_fm(wap, wb, 0, 128, lambda tb: (kwT[:, tb * 512:(tb + 1) * 512], B_kw), 4)
        self.proj_tm(wap, wb, 128, 128, lambda t: (vw[:, t, 0:128], B_vw[t]))
        self.proj_tm(wap, wb, 256, 12, lambda t: (gt[:, t, :], B_gt[t]), func=AF.Sigmoid)
        wap, wb = self.load_w(win[:, 3868:4380], 512)
        self.proj_tm(wap, wb, 0, 512, lambda t: (ztm[:, t, :], B_z[t]), func=AF.Silu)
        self.P.barrier()
        w1 = [self.wbuf[0][:, :, :].rearrange("p a b -> p (a b)").rearrange("p (a b) -> p a b", a=64),
              None]
        w1k = w1[0][:, 0:32, :]
        w1v = w1[0][:, 32:64, :]
        self.dma("pool", w1k, W["nsa_cmp_w1_k"].rearrange("(p d) h -> d p h", d=128), [], [self.B_w[0]])
        self.dma("pool", w1v, W["nsa_cmp_w1_v"].rearrange("(p d) h -> d p h", d=128), [], [self.B_w[0]])
        self.tr(ps[0][:, 0:64], posr[0:64, :], self.identf[0:64, 0:64], [B_pos, self.B_c], [Bp[0]])
        self.cp("dve", posT, ps[0][:, 0:64], [Bp[0]], [B_pos])
        for kv in range(2):
            w1x = w1k if kv == 0 else w1v
            src = kcT if kv == 0 else vcT
            Bs = B_kc if kv == 0 else B_vc
            for p in range(32):
                self.mm(ps[1][:, 0:1], w1x[:, p, :], posT[:, kv * 32 + p:kv * 32 + p + 1], p == 0, p == 31, [self.B_w[0], B_pos], [Bp[1]])
            self.cp("dve", cb[:, kv:kv + 1], ps[1][:, 0:1], [Bp[1]], [B_cb])
            for p in range(32):
                self.mm(ps[2][:, 0:127], w1x[:, p, :], src[:, p:p + 16 * 126 + 1:16], p == 0, p == 31, [self.B_w[0], Bs], [Bp[2]])
            self.act(hid[:, kv, 0:127], ps[2][:, 0:127], AF.Silu, [Bp[2], B_cb], [B_hid], bias=cb[:, kv:kv + 1])
        self.mm(ps[3][:, 0:127], w2k, hid[:, 0, 0:127], True, True, [B_w2, B_hid], [Bp[3]])
        self.cp("dve", kcmpT[:, 0:127], ps[3][:, 0:127], [Bp[3]], [B_kcmp])
        self.mm(ps[4][0:127, 0:128], hid[:, 1, 0:127], w2v, True, True, [B_w2, B_hid], [Bp[4]])
        self.cp("dve", vaug[0:127, 0:128], ps[4][0:127, 0:128], [Bp[4]], [B_vaug])
        self.P.barrier()
        self.wslot = 1
        self.prefetch(win[:, 4380:4892], 512, (l, 4380))
        self.off = mlp_off
        pS = A([2, 512], BF16)
        pC = A([2, 512], BF16)
        imp = A([32], F32)
        imp2 = A([32], F32)
        m8 = A([16], F32)
        nm = A([32], BF16)
        nmT = A([2, 4, 128], BF16)
        cfc = A([8], F32)
        cf = A([8], F32)
        ocs = A([2, 512], F32)
        osb = A([2, 512], F32)
        B_pS, B_pC, B_os, B_ocs, B_nmT = ([Buf(), Buf()] for _ in range(5))
        B_imp, B_imp2, B_m8, B_nm, B_cfc, B_cf = (Buf() for _ in range(6))
        self.tm_tail_alloc()
        self.memset("pool", nmT, 0.0, B_nmT)
        self.nsc = 0
        M1 = A([8, 32], F32)
        M2 = A([8, 32], F32)
        B_M = Buf()
        self.memset("pool", M1, 1.0, [B_M])
        self.memset("pool", M2, 0.0, [B_M])
        for T in range(8, NT):
            for hf in range(2):
                cur = 2 * T + hf
                sl = slice(hf * 64, (hf + 1) * 64)
                if cur + 1 < 32:
                    self.memset("pool", M1[sl, T - 8, cur + 1:32], 0.0, [B_M])
                    self.memset("pool", M2[sl, T - 8, cur + 1:32], NEG, [B_M])
                self.memset("pool", M1[sl, T - 8, 0:1], 0.0, [B_M])
                self.memset("pool", M2[sl, T - 8, 0:1], 1000.0, [B_M])
                self.memset("pool", M1[sl, T - 8, cur - 1:cur + 1], 0.0, [B_M])
                self.memset("pool", M2[sl, T - 8, cur - 1:cur + 1], 1000.0, [B_M])

        def qt_(T):
            return qT[:, :, T * 128:(T + 1) * 128]

        def oc(h, a, b):
            return ps[2 + h // 2][:, (h % 2) * 162 + a:(h % 2) * 162 + b]

        def A1(T):
            p = T % 2
            sb = self.nsc % 2
            self.mm(ps[sb][0:127, :], kcmpT[:, 0:127], qt_(T), True, True, [B_kcmp, B_q], [Bp[sb]])
            self.act(pC[0:127, p, :], ps[sb][0:127, :], AF.Exp, [Bp[sb]], [B_pC[p]])
            v = pC[0:127, p, :].rearrange("p (h t) -> p h t", h=4)
            self.asel(v, v, [[0, 4], [1, 128]], ALU.is_ge, 0.0, 128 * T - 31, -16, [B_pC[p]], [B_pC[p]])

        def A2(T):
            p = T % 2
            for h in range(4):
                bk = 2 + h // 2
                c0 = (h % 2) * 162
                self.mm(ps[bk][:, c0:c0 + 162], pC[0:127, p, h * 128:(h + 1) * 128], vaug[0:127, 0:162], h % 2 == 0, h % 2 == 1,
                        [B_pC[p], B_vaug], [Bp[bk]], skip_group_check=True)

        def A3(T):
            p = T % 2
            for bk in range(2):
                self.ts("dve", cfc[:, 2 * bk:2 * bk + 2], ps[2 + bk][:, 128:128 + 2 * 162:162], 1e-30, None, ALU.max, None, [Bp[2 + bk]], [B_cfc])
            self.P.op("dve", lambda e: e.reciprocal(out=cfc[:, 0:4], in_=cfc[:, 0:4]), reads=[B_cfc], writes=[B_cfc])
            if T >= 8:
                self.ts("dve", imp, oc(0, 130, 162), cfc[:, 0:1], None, ALU.mult, None, [Bp[2], B_cfc], [B_imp])
                for h in range(1, 4):
                    self.stt("dve", imp, oc(h, 130, 162), cfc[:, h:h + 1], imp, ALU.mult, ALU.add, [Bp[2 + h // 2], B_cfc, B_imp], [B_imp])
                self.tt("dve", imp, imp, M1[:, T - 8, :], ALU.mult, [B_imp, B_M], [B_imp])
                self.tt("dve", imp, imp, M2[:, T - 8, :], ALU.add, [B_imp, B_M], [B_imp])
                self.P.op("dve", lambda e: e.max(out=m8[:, 0:8], in_=imp), reads=[B_imp], writes=[B_m8])
                self.P.op("dve", lambda e: e.match_replace(out=imp2, in_to_replace=m8[:, 0:8], in_values=imp, imm_value=-2.0e30), reads=[B_imp, B_m8], writes=[B_imp2])
                self.P.op("dve", lambda e: e.max(out=m8[:, 8:16], in_=imp2), reads=[B_imp2], writes=[B_m8])
                self.ts("dve", nm, imp, m8[:, 15:16], 1.0, ALU.is_ge, ALU.subtract, [B_imp, B_m8], [B_nm])
            self.tt("dve", cfc[:, 4:8], cfc[:, 0:4], gt[:, T, 0:12:3], ALU.mult, [B_cfc, B_gt[T]], [B_cfc])
            for bk in range(2):
                src = ps[2 + bk][:, 0:324].rearrange("p (a b) -> p a b", a=2)[:, :, 0:128]
                self.tt("dve", ocs[:, p, bk * 256:(bk + 1) * 256].rearrange("p (a b) -> p a b", a=2), src,
                        cfc[:, 4 + 2 * bk:6 + 2 * bk].unsqueeze(2).to_broadcast([128, 2, 128]), ALU.mult, [Bp[2 + bk], B_cfc], [B_ocs[p]])

        def A4(T):
            p = T % 2
            if T >= 8:
                sb = self.nsc % 2
                pT = ps[sb].bitcast(BF16)
                self.tr(pT[0:32, 0:128], nm, self.ident, [B_nm, self.B_c], [Bp[sb]])
                self.cp("dve", nmT[0:32, p, :, :], pT[0:32, 0:128].unsqueeze(1).to_broadcast([32, 4, 128]), [Bp[sb]], [B_nmT[p]])

        def score(T, job):
            kind, kt = job
            p = T % 2
            i = self.nsc % 2
            self.nsc += 1
            use_sel = T >= 8
            if kind == "slc":
                self.mm(ps[i], ksT[:, kt * 128:(kt + 1) * 128], qt_(T), True, not use_sel, [B_ks, B_q], [Bp[i]])
                if use_sel:
                    self.mm(ps[i], EX[:, kt * 128:(kt + 1) * 128], nmT[:, p, :, :], False, True, [B_EX, B_nmT[p]], [Bp[i]])
            else:
                self.mm(ps[i], kwT[:, kt * 128:(kt + 1) * 128], qt_(T), True, True, [B_kw, B_q], [Bp[i]])
            self.act(pS[:, i, :], ps[i], AF.Exp, [Bp[i]], [B_pS[i]])
            v = pS[:, i, :].rearrange("p (h t) -> p h t", h=4)
            if kt == T:
                self.asel(v, v, [[0, 4], [1, 128]], ALU.is_ge, 0.0, 0, -1, [B_pS[i]], [B_pS[i]])
            if kind == "win" and kt == T - 4:
                self.asel(v, v, [[0, 4], [-1, 128]], ALU.is_ge, 0.0, -1, 1, [B_pS[i]], [B_pS[i]])
            return i

        def pv(T, job, i, first, last):
            kind, kt = job
            base = 4 if kind == "slc" else 6
            vv, Bv = (vs, B_vs) if kind == "slc" else (vw, B_vw)
            for h in range(4):
                bk = base + h // 2
                c0 = (h % 2) * 130
                self.mm(ps[bk][:, c0:c0 + 129], pS[:, i, h * 128:(h + 1) * 128], vv[:, kt, 0:129], first and h % 2 == 0, last and h % 2 == 1,
                        [B_pS[i], Bv[kt]], [Bp[bk]], skip_group_check=True)

        acc = A([4, 260], F32)
        B_acc = Buf()

        def C(T):
            p = T % 2
            for bk in range(4):
                self.act(acc[:, bk, :], ps[4 + bk][:, 0:260], AF.Copy, [Bp[4 + bk]], [B_acc])
            for bk in range(4):
                self.ts("dve", cf[:, 2 * bk:2 * bk + 2], acc[:, bk, 128:260:130], 1e-30, None, ALU.max, None, [B_acc], [B_cf])
            self.P.op("dve", lambda e: e.reciprocal(out=cf, in_=cf), reads=[B_cf], writes=[B_cf])
            self.tt("dve", cf.rearrange("p (b h) -> p b h", b=2), cf.rearrange("p (b h) -> p b h", b=2),
                    gt[:, T, :].rearrange("p (h b) -> p b h", b=3)[:, 1:3, :], ALU.mult, [B_cf, B_gt[T]], [B_cf])
            for h in range(4):
                o_h = osb[:, p, h * 128:(h + 1) * 128]
                self.stt("dve", o_h, acc[:, h // 2, (h % 2) * 130:(h % 2) * 130 + 128], cf[:, h:h + 1], ocs[:, p, h * 128:(h + 1) * 128], ALU.mult, ALU.add,
                         [B_acc, B_cf, B_ocs[p]], [B_os[p]])
                self.stt("dve", o_h, acc[:, 2 + h // 2, (h % 2) * 130:(h % 2) * 130 + 128], cf[:, 4 + h:5 + h], o_h, ALU.mult, ALU.add,
                         [B_acc, B_cf, B_os[p]], [B_os[p]])


        A1(0)
        A2(0)
        A3(0)
        A4(0)
        for T in range(NT):
            jobs = [("slc", kt) for kt in range(T + 1)] + [("win", kt) for kt in range(max(0, T - 4), T + 1)]
            firsts = {("slc", 0), ("win", max(0, T - 4))}
            nxt = T + 1 < NT
            slot = score(T, jobs[0])
            for n, job in enumerate(jobs):
                nslot = score(T, jobs[n + 1]) if n + 1 < len(jobs) else None
                if nxt and n == 0:
                    A1(T + 1)
                pv(T, job, slot, job in firsts, job[1] == T)
                if nxt and n == min(1, len(jobs) - 1):
                    A2(T + 1)
                    A3(T + 1)
                if T >= 1 and n == min(1, len(jobs) - 1):
                    self.tm_tail_a1(osb[:, (T - 1) % 2, :], B_os[(T - 1) % 2], T - 1)
                if T >= 2 and n == min(1, len(jobs) - 1):
                    self.tm_tail_b(2, T - 2, self.nsc % 2)
                slot = nslot
            if T >= 1:
                u = T - 1
                self.tm_tail_a2(u)
                self.tm_tail_a3(osb[:, u % 2, :], B_os[u % 2], ztm[:, u, :], B_z[u], 512, u)
            if nxt:
                A4(T + 1)
            C(T)
        u = NT - 1
        self.tm_tail_a1(osb[:, u % 2, :], B_os[u % 2], u)
        self.tm_tail_a2(u)
        self.tm_tail_a3(osb[:, u % 2, :], B_os[u % 2], ztm[:, u, :], B_z[u], 512, u)
        self.tm_tail_b(2, NT - 2, 0)
        self.tm_tail_b(2, NT - 1, 1)

    def outproj(self, l, src_d):
        A = self.alloc
        W = self.W
        ps, Bp = self.ps, self.B_ps
        mT = self.hT
        fuse = (l + 1 < self.L) and self.upto >= 5
        mview = self.mixT_d.rearrange("(k p) t -> p k t", p=128)
        if not getattr(self, "mT_pre", False):
            self.dma("sp", mT[:, 0:12, :], mview[:, 0:12, :], self.B_mixd[0:3] + self.B_hT, self.B_hT)
        self.mT_pre = False
        self.dma("sp", mT[:, 12:16, :], mview[:, 12:16, :], [self.B_mixd[3]] + self.B_hT, self.B_hT)
        wo = A([16, D], BF16)
        B_wo = [Buf() for _ in range(4)]
        for cbk in range(4):
            self.dma("pool", wo[:, :, cbk * 512:(cbk + 1) * 512], W["w_out"][:, cbk * 512:(cbk + 1) * 512].rearrange("(k p) c -> p k c", p=128), [], [B_wo[cbk]])
        self.dma("sp", self.gvec, W["post_norm_g"].partition_broadcast(128), [self.B_g], [self.B_g])
        if fuse:
            self.prefetch(self.w["w_in"][l + 1][:, 0:512], 512, (l + 1, 0))
            gpre = A([D], F32)
            B_gpre = Buf()
            self.dma("sp", gpre, self.w["pre_norm_g"][l + 1].partition_broadcast(128), [], [B_gpre])
        xt = A([D], F32)
        tmp = A([2, 512], F32)
        hn = A([D], BF16)
        st4 = A([NT, 4], F32)
        st = A([NT, 2], F32)
        B_xt, B_hn = Buf(), Buf()
        B_tmp = [Buf(), Buf()]
        B_st = [Buf() for _ in range(NT)]
        B_st4 = [Buf() for _ in range(NT)]
        def post(tp):
            bb = (tp % 2) * 4
            pA = ps[bb].bitcast(BF16)
            pB = ps[bb + 1].bitcast(BF16)
            for kc in range(16):
                dst = (pA if kc < 8 else pB)[:, (kc % 8) * 128:(kc % 8 + 1) * 128]
                self.tr(dst, hn[:, kc * 128:(kc + 1) * 128], self.ident, [B_hn, self.B_c], [Bp[bb + (0 if kc < 8 else 1)]])
            self.cp("act", self.hT[:, 0:8, tp * 128:(tp + 1) * 128], pA.rearrange("p (a b) -> p a b", a=8), [Bp[bb]], [self.B_hT[tp]])
            self.cp("dve", self.hT[:, 8:16, tp * 128:(tp + 1) * 128], pB.rearrange("p (a b) -> p a b", a=8), [Bp[bb + 1]], [self.B_hT[tp]])

        def ymm(t, cbk):
            b = (t % 2) * 4 + cbk
            for kc in range(16):
                self.mm(ps[b], mT[:, kc, t * 128:(t + 1) * 128], wo[:, kc, cbk * 512:(cbk + 1) * 512], kc == 0, kc == 15, [self.B_hT[t], B_wo[cbk]], [Bp[b]])

        for cbk in range(4):
            ymm(0, cbk)
            ymm(1, cbk)
        pend = None
        for t in range(NT):
            b0 = (t % 2) * 4
            self.dma("sp", xt, src_d[t * 128:(t + 1) * 128, :], [self.B_out[t]], [B_xt])
            if t >= 2:
                for cbk in range(4):
                    ymm(t, cbk)
            if pend is not None:
                post(pend)
                pend = None
            for cbk in range(4):
                b = b0 + cbk
                self.act(hn[:, cbk * 512:(cbk + 1) * 512], ps[b], AF.Square, [Bp[b]], [B_hn, B_st4[t]], accum_out=st4[:, t, cbk:cbk + 1])
            ss = st[:, t, 0:1]
            self.P.op("dve", lambda e, ss=ss, t=t: e.tensor_reduce(out=ss, in_=st4[:, t, :], axis=AX.X, op=ALU.add), reads=[B_st4[t]], writes=[B_st[t]])
            self.ts("dve", ss, ss, 1.0 / D, 1e-6, ALU.mult, ALU.add, [B_st[t]], [B_st[t]])
            self.rsqrt(ss, B_st[t])
            for cbk in range(4):
                b = b0 + cbk
                j = cbk % 2
                cs = slice(cbk * 512, (cbk + 1) * 512)
                self.stt("dve", tmp[:, j, :], ps[b], ss, self.gvec[:, cs], ALU.mult, ALU.mult, [Bp[b], B_st[t], self.B_g], [B_tmp[j]])
                self.tt("pool" if j else "dve", xt[:, cs], xt[:, cs], tmp[:, j, :], ALU.add, [B_xt, B_tmp[j]], [B_xt])
            self.dma("sp", self.out_d[t * 128:(t + 1) * 128, :], xt, [B_xt], [self.B_out[t]])
            if fuse:
                s2 = st[:, t, 1:2]
                self.act(hn, xt, AF.Square, [B_xt], [B_hn, B_st[t]], accum_out=s2)
                self.ts("dve", s2, s2, 1.0 / D, 1e-6, ALU.mult, ALU.add, [B_st[t]], [B_st[t]])
                self.rsqrt(s2, B_st[t])
                self.stt("dve", hn, xt, s2, gpre, ALU.mult, ALU.mult, [B_xt, B_st[t], B_gpre], [B_hn])
                pend = t
        if pend is not None:
            post(pend)


def build_nc(L=4, upto=99, dbg=False):
    nc = bass.Bass("TRN2", target_bir_lowering=False)
    Builder(nc, L, upto, dbg).build()
    return nc


def kernel(**inputs):
    L = 4
    nc = build_nc(L)
    x = np.ascontiguousarray(inputs["x"], dtype=np.float32)
    B = x.shape[0]
    in_maps = []
    for b in range(B):
        m = {"x": x[b]}
        for n, _ in WNAMES:
            m[n] = np.ascontiguousarray(inputs[n], dtype=np.float32)
        in_maps.append(m)
    res = run_bass_kernel_spmd(nc, in_maps, core_ids=list(range(B)))
    return np.stack([res.results[b]["out"] for b in range(B)], axis=0).astype(np.float32)
```
